# Optimizing a Trainium2 kernel written in Bass

```python
import math
import jax
import jax.numpy as jnp
from jax import lax
import numpy as np


D_MODEL = 1024
BATCH = 16
SEQ = 2048
DEPTH = 1

ATT_HEADS = 8
ATT_HEAD_DIM = 64
ATT_QK_WIDTH = ATT_HEADS * 2 * ATT_HEAD_DIM
ATT_V_WIDTH = ATT_HEADS * 2 * ATT_HEAD_DIM
ROPE_THETA = 10000.0
Q_BLOCK = 128
SSD_EXPAND = 2
SSD_INNER = SSD_EXPAND * D_MODEL
SSD_HEAD_DIM = 64
SSD_HEADS = SSD_INNER // SSD_HEAD_DIM
SSD_GROUPS = 8
SSD_HEADS_PER_GROUP = SSD_HEADS // SSD_GROUPS
SSD_STATE = 128
SSD_CONV = 4
SSD_CONV_CH = SSD_INNER + 2 * SSD_GROUPS * SSD_STATE
SSD_CHUNK = 128
IN_SPLITS = (ATT_QK_WIDTH, ATT_QK_WIDTH, ATT_V_WIDTH, SSD_INNER, SSD_CONV_CH, SSD_HEADS, D_MODEL, D_MODEL)
IN_COLS = sum(IN_SPLITS)
MOE_GROUPS = 8
MOE_EXPERTS_PER_GROUP = 8
MOE_EXPERTS = MOE_GROUPS * MOE_EXPERTS_PER_GROUP
MOE_TOP_K = 2
MOE_HIDDEN = 512
MOE_BLOCK = 128
NORM_EPS = 1e-6
SUBLN_EPS = 1e-5
SSD_NORM_EPS = 1e-5

kernel_name = 'hybrid_diffattn_ssd_hiermoe_block'


def rms_norm(x, w, eps):
    xf = x.astype(jnp.float32)
    y = xf * lax.rsqrt(jnp.mean(xf * xf, axis=-1, keepdims=True) + eps)
    return (y * w.astype(jnp.float32)).astype(x.dtype)


def rotary_tables(positions):
    inv_freq = 1.0 / (ROPE_THETA ** (jnp.arange(0, ATT_HEAD_DIM, 2, dtype=jnp.float32) / ATT_HEAD_DIM))
    ang = positions.astype(jnp.float32)[..., None] * inv_freq
    return jnp.cos(ang), jnp.sin(ang)


def apply_rotary(t, cos, sin):
    c = cos[:, :, None, None, :]
    sn = sin[:, :, None, None, :]
    tf = t.astype(jnp.float32)
    t1, t2 = jnp.split(tf, 2, axis=-1)
    return jnp.concatenate([t1 * c - t2 * sn, t2 * c + t1 * sn], axis=-1).astype(t.dtype)


def diff_attention(q, k, v, lam):
    s = q.shape[1]
    scale = ATT_HEAD_DIM ** -0.5
    outs = []
    for blk in range(s // Q_BLOCK):
        q_lo = blk * Q_BLOCK
        q_hi = q_lo + Q_BLOCK
        scores = jnp.einsum('bqhcd,bkhcd->bhcqk', q[:, q_lo:q_hi], k[:, :q_hi],
                            preferred_element_type=jnp.float32) * scale
        causal = (q_lo + jnp.arange(Q_BLOCK))[:, None] >= jnp.arange(q_hi)[None, :]
        probs = jax.nn.softmax(jnp.where(causal, scores, -jnp.inf), axis=-1)
        weights = probs[:, :, 0] - lam * probs[:, :, 1]
        outs.append(jnp.einsum('bhqk,bkhe->bqhe', weights.astype(v.dtype), v[:, :q_hi]))
    return jnp.concatenate(outs, axis=1)


def causal_depthwise_conv(u, w, bias):
    out = lax.conv_general_dilated(u, w[:, None, :], window_strides=(1,), padding=[(SSD_CONV - 1, 0)],
                                   dimension_numbers=('NWC', 'WIO', 'NWC'),
                                   feature_group_count=u.shape[-1])
    return out + bias


def ssd_chunked_scan(xdt, a, bm, cm):
    b, s, g, r, p = xdt.shape
    n = bm.shape[-1]
    nc = s // SSD_CHUNK

    def to_chunks(t):
        return jnp.moveaxis(t.reshape(b, nc, SSD_CHUNK, *t.shape[2:]), 1, 0)

    causal = jnp.tril(jnp.ones((SSD_CHUNK, SSD_CHUNK), dtype=bool))

    def step(state, inp):
        xc, ac, bc, cc = inp
        cum = jnp.cumsum(ac, axis=1)
        cum_t = jnp.moveaxis(cum, 1, -1)
        seg = cum_t[..., :, None] - cum_t[..., None, :]
        decay = jnp.exp(jnp.where(causal, seg, -jnp.inf))
        cb = jnp.einsum('bign,bjgn->bgij', cc, bc)
        y_diag = jnp.einsum('bgij,bgrij,bjgrp->bigrp', cb, decay, xc)
        y_off = jnp.einsum('bign,bgrpn->bigrp', cc, state) * jnp.exp(cum)[..., None]
        total = cum[:, -1]
        w_tail = jnp.exp(total[:, None] - cum)
        new_state = state * jnp.exp(total)[..., None, None] + jnp.einsum('bjgn,bjgr,bjgrp->bgrpn', bc, w_tail, xc)
        return new_state, y_diag + y_off

    init = jnp.zeros((b, g, r, p, n), jnp.float32)
    _, ys = lax.scan(step, init, (to_chunks(xdt), to_chunks(a), to_chunks(bm), to_chunks(cm)))
    return jnp.moveaxis(ys, 0, 1).reshape(b, s, g, r, p)


def ssd_branch(z, xbc, dt_raw, conv_w, conv_b, dt_bias, a_log, d_skip, norm_w):
    b, s, _ = z.shape
    xbc = jax.nn.silu(causal_depthwise_conv(xbc, conv_w, conv_b))
    xs, bm, cm = jnp.split(xbc, [SSD_INNER, SSD_INNER + SSD_GROUPS * SSD_STATE], axis=-1)
    xs = xs.reshape(b, s, SSD_GROUPS, SSD_HEADS_PER_GROUP, SSD_HEAD_DIM).astype(jnp.float32)
    bm = bm.reshape(b, s, SSD_GROUPS, SSD_STATE).astype(jnp.float32)
    cm = cm.reshape(b, s, SSD_GROUPS, SSD_STATE).astype(jnp.float32)
    dt = jax.nn.softplus(dt_raw.astype(jnp.float32) + dt_bias.astype(jnp.float32))
    dt = dt.reshape(b, s, SSD_GROUPS, SSD_HEADS_PER_GROUP)
    a = -jnp.exp(a_log.astype(jnp.float32)).reshape(SSD_GROUPS, SSD_HEADS_PER_GROUP)
    dsk = d_skip.astype(jnp.float32).reshape(SSD_GROUPS, SSD_HEADS_PER_GROUP)
    y = ssd_chunked_scan(xs * dt[..., None], dt * a, bm, cm) + dsk[..., None] * xs
    y = y.reshape(b, s, SSD_INNER) * jax.nn.silu(z.astype(jnp.float32))
    yg = y.reshape(b, s, SSD_GROUPS, SSD_INNER // SSD_GROUPS)
    yg = yg * lax.rsqrt(jnp.mean(yg * yg, axis=-1, keepdims=True) + SSD_NORM_EPS)
    return (yg.reshape(b, s, SSD_INNER) * norm_w.astype(jnp.float32)).astype(z.dtype)


def hierarchical_moe(h, w_gr, b_gr, w_er, b_er, w_gate, w_up, w_down):
    b, s, d = h.shape
    t = h.reshape(-1, d)
    n_tok = t.shape[0]
    g_logits = jnp.matmul(t, w_gr).astype(jnp.float32) + b_gr.astype(jnp.float32)
    g_prob = jax.nn.softmax(g_logits, axis=-1)
    g_sel = jnp.argmax(g_logits, axis=-1)
    g_gate = jnp.take_along_axis(g_prob, g_sel[:, None], axis=-1)[:, 0]
    e_logits = (jnp.matmul(t, w_er).astype(jnp.float32) + b_er.astype(jnp.float32))
    e_logits = e_logits.reshape(n_tok, MOE_GROUPS, MOE_EXPERTS_PER_GROUP)
    e_sel_logits = jnp.take_along_axis(e_logits, g_sel[:, None, None], axis=1)[:, 0]
    top_v, top_i = lax.top_k(e_sel_logits, MOE_TOP_K)
    weights = g_gate[:, None] * jax.nn.softmax(top_v, axis=-1)
    eid = g_sel[:, None].astype(jnp.int32) * MOE_EXPERTS_PER_GROUP + top_i.astype(jnp.int32)
    n_assign = n_tok * MOE_TOP_K
    flat_e = eid.reshape(-1)
    flat_tok = jnp.repeat(jnp.arange(n_tok, dtype=jnp.int32), MOE_TOP_K)
    flat_w = weights.reshape(-1)
    order = jnp.argsort(flat_e)
    se, st, sw = flat_e[order], flat_tok[order], flat_w[order]
    counts = jnp.bincount(flat_e, length=MOE_EXPERTS)
    starts = jnp.cumsum(counts) - counts
    padded = ((counts + MOE_BLOCK - 1) // MOE_BLOCK) * MOE_BLOCK
    pad_end = jnp.cumsum(padded)
    pad_start = pad_end - padded
    dest = pad_start[se] + (jnp.arange(n_assign, dtype=jnp.int32) - starts[se])
    n_buf = n_assign + MOE_EXPERTS * MOE_BLOCK
    n_blocks = n_buf // MOE_BLOCK
    buf_tok = jnp.zeros((n_buf,), jnp.int32).at[dest].set(st)
    buf_w = jnp.zeros((n_buf,), jnp.float32).at[dest].set(sw)
    blk_expert = jnp.minimum(jnp.searchsorted(pad_end, jnp.arange(n_blocks) * MOE_BLOCK, side='right'),
                             MOE_EXPERTS - 1)
    xb = t[buf_tok].reshape(n_blocks, MOE_BLOCK, d)

    def expert_block(args):
        xblk, e = args
        hid = jax.nn.silu(xblk @ w_gate[e]) * (xblk @ w_up[e])
        return hid @ w_down[e]

    yb = lax.map(expert_block, (xb, blk_expert)).reshape(n_buf, d)
    out = jnp.zeros((n_tok, d), jnp.float32).at[buf_tok].add(yb.astype(jnp.float32) * buf_w[:, None])
    return out.astype(h.dtype).reshape(b, s, d)


def setup_inputs(seed: int = 0) -> dict:
    key = jax.random.key(seed)
    ks = jax.random.split(key, 28)
    f32 = jnp.float32

    def normal(k, shape, scale):
        return jax.random.normal(k, shape, f32) * scale

    x = normal(ks[0], (BATCH, SEQ, D_MODEL), 1.0)
    positions = (jnp.arange(SEQ, dtype=jnp.int32)[None, :]
                 + jax.random.randint(ks[1], (BATCH, 1), 0, SEQ, dtype=jnp.int32))
    norm_mix_w = 1.0 + normal(ks[2], (DEPTH, D_MODEL), 0.02)
    w_in = normal(ks[3], (DEPTH, D_MODEL, IN_COLS), D_MODEL ** -0.5)
    conv_w = normal(ks[4], (DEPTH, SSD_CONV, SSD_CONV_CH), SSD_CONV ** -0.5)
    conv_b = normal(ks[5], (DEPTH, SSD_CONV_CH), 0.02)
    dt0 = jnp.exp(jax.random.uniform(ks[6], (DEPTH, SSD_HEADS), f32, math.log(1e-3), math.log(1e-1)))
    dt_bias = dt0 + jnp.log(-jnp.expm1(-dt0))
    a_log = jnp.log(jax.random.uniform(ks[7], (DEPTH, SSD_HEADS), f32, 1.0, 16.0))
    d_skip = 1.0 + normal(ks[8], (DEPTH, SSD_HEADS), 0.1)
    ssd_norm_w = 1.0 + normal(ks[9], (DEPTH, SSD_INNER), 0.02)
    lambda_q1 = normal(ks[10], (DEPTH, ATT_HEAD_DIM), 0.1)
    lambda_k1 = normal(ks[11], (DEPTH, ATT_HEAD_DIM), 0.1)
    lambda_q2 = normal(ks[12], (DEPTH, ATT_HEAD_DIM), 0.1)
    lambda_k2 = normal(ks[13], (DEPTH, ATT_HEAD_DIM), 0.1)
    subln_w = 1.0 + normal(ks[14], (DEPTH, 2 * ATT_HEAD_DIM), 0.02)
    w_branch_attn = normal(ks[15], (DEPTH, ATT_V_WIDTH, D_MODEL), ATT_V_WIDTH ** -0.5)
    w_branch_ssd = normal(ks[16], (DEPTH, SSD_INNER, D_MODEL), SSD_INNER ** -0.5)
    w_out = normal(ks[17], (DEPTH, D_MODEL, D_MODEL), D_MODEL ** -0.5)
    norm_ffn_w = 1.0 + normal(ks[18], (DEPTH, D_MODEL), 0.02)
    w_group_router = normal(ks[19], (DEPTH, D_MODEL, MOE_GROUPS), D_MODEL ** -0.5)
    b_group_router = normal(ks[20], (DEPTH, MOE_GROUPS), 0.01)
    w_expert_router = normal(ks[21], (DEPTH, D_MODEL, MOE_EXPERTS), D_MODEL ** -0.5)
    b_expert_router = normal(ks[22], (DEPTH, MOE_EXPERTS), 0.01)
    w_expert_gate = normal(ks[23], (DEPTH, MOE_EXPERTS, D_MODEL, MOE_HIDDEN), D_MODEL ** -0.5)
    w_expert_up = normal(ks[24], (DEPTH, MOE_EXPERTS, D_MODEL, MOE_HIDDEN), D_MODEL ** -0.5)
    w_expert_down = normal(ks[25], (DEPTH, MOE_EXPERTS, MOE_HIDDEN, D_MODEL), MOE_HIDDEN ** -0.5)
    final_norm_w = 1.0 + normal(ks[26], (D_MODEL,), 0.02)
    return {'x': x, 'positions': positions, 'norm_mix_w': norm_mix_w, 'w_in': w_in,
            'conv_w': conv_w, 'conv_b': conv_b, 'dt_bias': dt_bias, 'a_log': a_log, 'd_skip': d_skip,
            'ssd_norm_w': ssd_norm_w, 'lambda_q1': lambda_q1, 'lambda_k1': lambda_k1,
            'lambda_q2': lambda_q2, 'lambda_k2': lambda_k2, 'subln_w': subln_w,
            'w_branch_attn': w_branch_attn, 'w_branch_ssd': w_branch_ssd, 'w_out': w_out,
            'norm_ffn_w': norm_ffn_w, 'w_group_router': w_group_router, 'b_group_router': b_group_router,
            'w_expert_router': w_expert_router, 'b_expert_router': b_expert_router,
            'w_expert_gate': w_expert_gate, 'w_expert_up': w_expert_up, 'w_expert_down': w_expert_down,
            'final_norm_w': final_norm_w}


def reference(x, positions, norm_mix_w, w_in, conv_w, conv_b, dt_bias, a_log, d_skip, ssd_norm_w,
              lambda_q1, lambda_k1, lambda_q2, lambda_k2, subln_w, w_branch_attn, w_branch_ssd, w_out,
              norm_ffn_w, w_group_router, b_group_router, w_expert_router, b_expert_router,
              w_expert_gate, w_expert_up, w_expert_down, final_norm_w):
    b, s, _ = x.shape
    cos, sin = rotary_tables(positions)
    split_at = [int(o) for o in np.cumsum(IN_SPLITS)[:-1]]
    for l in range(DEPTH):
        lam_init = 0.8 - 0.6 * math.exp(-0.3 * l)
        h = rms_norm(x, norm_mix_w[l], NORM_EPS)
        proj = h @ w_in[l]
        q, k, v, z, xbc, dt_raw, gate_a, gate_s = jnp.split(proj, split_at, axis=-1)
        q = apply_rotary(q.reshape(b, s, ATT_HEADS, 2, ATT_HEAD_DIM), cos, sin)
        k = apply_rotary(k.reshape(b, s, ATT_HEADS, 2, ATT_HEAD_DIM), cos, sin)
        v = v.reshape(b, s, ATT_HEADS, 2 * ATT_HEAD_DIM)
        lam = (jnp.exp(jnp.sum(lambda_q1[l].astype(jnp.float32) * lambda_k1[l].astype(jnp.float32)))
               - jnp.exp(jnp.sum(lambda_q2[l].astype(jnp.float32) * lambda_k2[l].astype(jnp.float32)))
               + lam_init)
        att = diff_attention(q, k, v, lam)
        att = (rms_norm(att, subln_w[l], SUBLN_EPS) * (1.0 - lam_init)).reshape(b, s, ATT_V_WIDTH)
        ssd = ssd_branch(z, xbc, dt_raw, conv_w[l], conv_b[l], dt_bias[l], a_log[l], d_skip[l], ssd_norm_w[l])
        merged = (jax.nn.sigmoid(gate_a) * (att @ w_branch_attn[l])
                  + jax.nn.sigmoid(gate_s) * (ssd @ w_branch_ssd[l]))
        x = x + merged @ w_out[l]
        h2 = rms_norm(x, norm_ffn_w[l], NORM_EPS)
        x = x + hierarchical_moe(h2, w_group_router[l], b_group_router[l], w_expert_router[l],
                                 b_expert_router[l], w_expert_gate[l], w_expert_up[l], w_expert_down[l])
    return rms_norm(x, final_norm_w, NORM_EPS)
```

```python
import contextlib
import math
import numpy as np
import concourse.bass as bass
import concourse.mybir as mybir
from concourse.bass_utils import run_bass_kernel_spmd

F32 = mybir.dt.float32
BF16 = mybir.dt.bfloat16
I32 = mybir.dt.int32
ALU = mybir.AluOpType
AF = mybir.ActivationFunctionType
AX = mybir.AxisListType

NCORES = 8
TOK = 4096
NT = 32
D = 1024
WCOLS = 13344
PI = math.pi


class Buf:
    __slots__ = ("name", "writers", "readers", "lane", "swlane", "is_dram")

    def __init__(self, name):
        self.name = name
        self.is_dram = False
        self.swlane = None
        self.writers = []
        self.readers = []
        self.lane = None


class Op:
    __slots__ = ("eng", "fn", "deps", "signaled", "is_dma", "token", "lane")

    def __init__(self, eng, fn, is_dma):
        self.eng = eng
        self.fn = fn
        self.deps = set()
        self.signaled = False
        self.is_dma = is_dma
        self.token = None
        self.lane = None


class Sched:
    ENGS = ("pe", "act", "dve", "pool", "sp")

    def __init__(self, nc, st, nlanes=56):
        self.nc = nc
        self.ops = []
        self.allbufs = []
        self.esem = {e: st.enter_context(nc.semaphore(f"s_{e}")) for e in ("pe", "act", "dve", "pool")}
        self.ecount = {e: 0 for e in self.esem}
        self.bar = st.enter_context(nc.semaphore("s_bar"))
        self.barcount = 0
        self.lanes = [[st.enter_context(nc.semaphore(f"l{i}")), 0] for i in range(nlanes)]
        self.swlanes = [[st.enter_context(nc.semaphore(f"w{i}")), 0] for i in range(8)]
        self.waited = {e: {} for e in self.ENGS}
        self.eng = {"pe": nc.tensor, "act": nc.scalar, "dve": nc.vector, "pool": nc.gpsimd, "sp": nc.sync}
        self.nins = 0
        self.douts = {}

    def buf(self, name="b"):
        b = Buf(name)
        self.allbufs.append(b)
        return b

    def dout(self, name):
        if name not in self.douts:
            self.douts[name] = self.buf(name)
            self.douts[name].is_dram = True
        return self.douts[name]

    def bufs(self, n, name="b"):
        return [self.buf(f"{name}{i}") for i in range(n)]

    def op(self, eng, fn, reads=(), writes=(), dma=False, waw=True):
        o = Op(eng, fn, dma)
        oid = len(self.ops)
        for b in reads:
            o.deps.update(b.writers)
        for b in writes:
            o.deps.update(b.readers)
            if waw:
                o.deps.update(b.writers)
        for b in reads:
            b.readers.append(oid)
        for b in writes:
            if waw:
                b.writers = [oid]
                b.readers = []
            else:
                b.writers.append(oid)
        if dma:
            o.lane = reads[0] if writes[0].is_dram else writes[0]
            assert not o.lane.is_dram
        o.deps.discard(oid)
        self.ops.append(o)
        return oid

    def seq(self, eng, fns, reads=(), writes=()):
        for fn in fns:
            self.op(eng, fn, reads, writes)

    def pe(self, fn, reads=(), writes=()):
        return self.op("pe", fn, reads, writes)

    def act(self, fn, reads=(), writes=()):
        return self.op("act", fn, reads, writes)

    def dve(self, fn, reads=(), writes=()):
        return self.op("dve", fn, reads, writes)

    def pool(self, fn, reads=(), writes=()):
        return self.op("pool", fn, reads, writes)

    def dma(self, eng, fn, reads=(), writes=(), waw=True):
        return self.op(eng, fn, reads, writes, dma=True, waw=waw)

    def flush(self, final=False):
        ops = self.ops
        for o in ops:
            for d in o.deps:
                ops[d].signaled = True
        last = {}
        for oid, o in enumerate(ops):
            if not o.is_dma:
                last[o.eng] = oid
        for oid in last.values():
            ops[oid].signaled = True
        free_lanes = list(range(len(self.lanes)))
        free_sw = list(range(len(self.swlanes)))
        used_lanes = []
        for oid, o in enumerate(ops):
            eo = self.eng[o.eng]
            need = {}
            for d in o.deps:
                dop = ops[d]
                if (not dop.is_dma) and dop.eng == "pe" and o.eng == "pe" and not o.is_dma:
                    continue
                sem, val = dop.token
                k = id(sem)
                if k not in need or need[k][1] < val:
                    need[k] = (sem, val)
            for k, (sem, val) in need.items():
                if self.waited[o.eng].get(k, 0) >= val:
                    continue
                eo.wait_ge(sem, val)
                self.waited[o.eng][k] = val
            ins = o.fn(eo)
            self.nins += 1
            if o.is_dma:
                lb = o.lane
                if o.eng == "pool":
                    if lb.swlane is None:
                        lb.swlane = free_sw.pop(0)
                        used_lanes.append(self.swlanes[lb.swlane])
                    ln = self.swlanes[lb.swlane]
                else:
                    if lb.lane is None:
                        lb.lane = free_lanes.pop(0)
                        used_lanes.append(self.lanes[lb.lane])
                    ln = self.lanes[lb.lane]
                ln[1] += 16
                ins.then_inc(ln[0], 16)
                o.token = (ln[0], ln[1])
            elif o.signaled:
                self.ecount[o.eng] += 1
                ins.then_inc(self.esem[o.eng], 1)
                o.token = (self.esem[o.eng], self.ecount[o.eng])
        sp = self.nc.sync
        for e in ("pe", "act", "dve", "pool"):
            if self.ecount[e] > 0:
                sp.wait_ge(self.esem[e], self.ecount[e])
        for ln in used_lanes:
            sp.wait_ge(ln[0], ln[1])
        if not final:
            self.barcount += 1
            sp.sem_inc(self.bar, 1)
            for e in ("pe", "act", "dve", "pool"):
                self.eng[e].wait_ge(self.bar, self.barcount)
        self.ops = []
        for b in self.allbufs:
            b.writers = []
            b.readers = []
            b.lane = None
            b.swlane = None


def bc3(ap2, n):
    p, a = ap2.shape
    return ap2.unsqueeze(2).to_broadcast([p, a, n])


def bcmid(ap2, n):
    p, a = ap2.shape
    return ap2.unsqueeze(1).to_broadcast([p, n, a])


def build_program(debug=False, stop_after=99):
    nc = bass.Bass("TRN2", target_bir_lowering=False)

    def din(name, shape, dt=F32):
        return nc.dram_tensor(name, list(shape), dt, kind="ExternalInput").ap()

    def dscr(name, shape, dt=F32):
        kind = "ExternalOutput" if debug else "Internal"
        return nc.dram_tensor(name, list(shape), dt, kind=kind).ap()

    x = din("x", [TOK, D])
    pos = din("pos", [TOK], I32)
    w_in = din("w_in", [D, WCOLS])
    normw1 = din("normw1", [128, 8])
    convw = din("convw", [128, 32, 4])
    convb = din("convb", [128, 32])
    dt_bias = din("dt_bias", [32])
    a_log = din("a_log", [32])
    d_skip = din("d_skip", [32])
    ssd_nw = din("ssd_nw", [2048])
    lam4 = din("lam4", [256])
    subln = din("subln", [128])
    w_pa = din("w_pa", [1024, 1024])
    w_ps = din("w_ps", [2048, 1024])
    w_o = din("w_o", [1024, 1024])
    normw2 = din("normw2", [1024])
    w_r = din("w_r", [1024, 72])
    b_r = din("b_r", [72])
    if stop_after > 5:
        w_g = din("w_g", [8192, 4096])
        w_u = din("w_u", [8192, 4096])
        w_d = din("w_d", [8192, 4096])
    fnw = din("fnw", [1024])
    rconst = din("rconst", [128, 2])
    out = nc.dram_tensor("out", [TOK, D], F32, kind="ExternalOutput").ap()

    QT = dscr("QT", [8, 128, TOK], BF16)
    KT = dscr("KT", [8, 128, TOK], BF16)
    XBCT = dscr("XBCT", [32, 128, TOK], BF16)
    VT = dscr("VT", [TOK, 1024], BF16)
    ZS = dscr("ZS", [TOK, 2048], BF16)
    GA = dscr("GA", [TOK, 1024], BF16)
    GS = dscr("GS", [TOK, 1024], BF16)
    DTR = dscr("DTR", [TOK, 32], F32)
    ATT = dscr("ATT", [TOK, 1024], BF16)
    SSD = dscr("SSD", [TOK, 2048], BF16)
    X1 = dscr("X1", [TOK, D], F32)
    H2 = dscr("H2", [TOK, D], F32)
    RT = dscr("RT", [128, 32, 8], F32)
    XBUF = nc.dram_tensor("XBUF", [16384, D], F32, kind="Internal").ap()
    YBUF = nc.dram_tensor("YBUF", [16384, D], F32, kind="Internal").ap()

    with contextlib.ExitStack() as gst:
        S = Sched(nc, gst)

        def sbg(name, shape, dt):
            return gst.enter_context(nc.sbuf_tensor(name, list(shape), dt))

        ident_f = sbg("ident_f", [128, 128], F32)
        ident_b = sbg("ident_b", [128, 128], BF16)
        ones_f = sbg("ones_f", [128, 128], F32)
        ones_b = sbg("ones_b", [128, 128], BF16)
        tri_f = sbg("tri_f", [128, 128], F32)
        striu_b = sbg("striu_b", [128, 128], BF16)
        negmask = sbg("negmask", [128, 128], F32)
        zeros_f = sbg("zeros_f", [128, 128], F32)
        hT_all = None

        def consts():
            bC = S.buf("consts")

            S.seq("pool", [
                lambda e: e.memset(ones_f[:], 1.0),
                lambda e: e.memset(zeros_f[:], 0.0),
                lambda e: e.affine_select(out=ident_f[:], in_=ones_f[:], pattern=[[-1, 128]], compare_op=ALU.is_equal,
                                          fill=0.0, base=0, channel_multiplier=1),
                lambda e: e.affine_select(out=tri_f[:], in_=ones_f[:], pattern=[[1, 128]], compare_op=ALU.is_ge,
                                          fill=0.0, base=0, channel_multiplier=-1),
                lambda e: e.affine_select(out=negmask[:], in_=zeros_f[:], pattern=[[1, 128]], compare_op=ALU.is_ge,
                                          fill=-30000.0, base=0, channel_multiplier=-1),
                lambda e: e.affine_select(out=striu_b[:], in_=ones_f[:], pattern=[[1, 128]], compare_op=ALU.is_gt,
                                          fill=0.0, base=0, channel_multiplier=-1),
                lambda e: e.tensor_copy(out=ident_b[:], in_=ident_f[:]),
                lambda e: e.tensor_copy(out=ones_b[:], in_=ones_f[:]),
            ], writes=[bC])
            S.flush()
        consts()

        with contextlib.ExitStack() as st:
            def sb(name, shape, dt):
                return st.enter_context(nc.sbuf_tensor(name, list(shape), dt))

            def ps(name, shape, dt):
                return st.enter_context(nc.psum_tensor(name, list(shape), dt))

            hT = sb("hT_all", [128, 8, TOK], BF16)
            Ct = sb("Ct", [128, TOK], F32)
            St = sb("St", [128, TOK], F32)
            st1x = contextlib.ExitStack()
            sb_o, ps_o = sb, ps
            sb = lambda name, shape, dt: st1x.enter_context(nc.sbuf_tensor(name, list(shape), dt))
            ps = lambda name, shape, dt: st1x.enter_context(nc.psum_tensor(name, list(shape), dt))
            b_hT = S.bufs(NT, "hT")
            xt = [sb(f"xt{i}", [128, D], F32) for i in range(2)]
            b_xt = S.bufs(2, "xt")
            hb = [sb(f"hb{i}", [128, D], BF16) for i in range(2)]
            b_hb = S.bufs(2, "hb")
            junk = sb("junk", [128, D], BF16)
            b_junk = S.buf("junk")
            st1 = sb("st1", [128, NT, 4], F32)
            b_st1 = S.bufs(NT, "st1")
            ptp = [ps(f"ptp{i}", [128, 8, 128], BF16) for i in range(2)]
            b_ptp = S.bufs(2, "ptp")
            for i in range(NT):
                u = i % 2
                S.dma("sp", lambda e, i=i, u=u: e.dma_start(out=xt[u][:], in_=x[i * 128:(i + 1) * 128, :]), writes=[b_xt[u]])
                S.act(lambda e, i=i, u=u: e.activation(out=junk[:], in_=xt[u][:], func=AF.Square, accum_out=st1[:, i, 0:1]),
                      reads=[b_xt[u]], writes=[b_junk, b_st1[i]])
                S.dve(lambda e, i=i: e.tensor_scalar(out=st1[:, i, 1:2], in0=st1[:, i, 0:1], scalar1=1.0 / D, scalar2=1e-6,
                                                     op0=ALU.mult, op1=ALU.add), reads=[b_st1[i]], writes=[b_st1[i]])
                S.act(lambda e, i=i: e.activation(out=st1[:, i, 2:3], in_=st1[:, i, 1:2], func=AF.Sqrt), reads=[b_st1[i]], writes=[b_st1[i]])
                S.dve(lambda e, i=i: e.reciprocal(out=st1[:, i, 3:4], in_=st1[:, i, 2:3]), reads=[b_st1[i]], writes=[b_st1[i]])
                S.dve(lambda e, i=i, u=u: e.tensor_scalar(out=hb[u][:], in0=xt[u][:], scalar1=st1[:, i, 3:4], scalar2=None, op0=ALU.mult),
                      reads=[b_xt[u], b_st1[i]], writes=[b_hb[u]])

                def tp(e, u=u):
                    r = None
                    for kc in range(8):
                        r = e.transpose(out=ptp[u][:, kc, :], in_=hb[u][:, kc * 128:(kc + 1) * 128], identity=ident_b[:])
                    return r
                S.pe(tp, reads=[b_hb[u]], writes=[b_ptp[u]])
                S.act(lambda e, i=i, u=u: e.copy(out=hT[:, :, i * 128:(i + 1) * 128], in_=ptp[u][:]), reads=[b_ptp[u]], writes=[b_hT[i]])

            rc = sb("rc", [128, 2], F32)
            b_rc = S.buf("rc")
            posi = sb("posi", [128, TOK], I32)
            ang = sb("ang", [128, TOK], F32)
            tmpa = sb("tmpa", [128, TOK], F32)
            tmpb = sb("tmpb", [128, TOK], F32)
            negpi = sb("negpi", [128, 1], F32)
            b_rot = S.buf("rot")
            S.dma("sp", lambda e: e.dma_start(out=rc[:], in_=rconst), writes=[b_rc])
            S.dma("sp", lambda e: e.dma_start(out=posi[:], in_=pos.partition_broadcast(128)), writes=[b_rot])

            rot_fns = [lambda e: e.memset(negpi[:], -PI),
                       lambda e: e.tensor_copy(out=ang[:], in_=posi[:]),
                       lambda e: e.tensor_scalar(out=ang[:], in0=ang[:], scalar1=rc[:, 0:1], scalar2=None, op0=ALU.mult)]
            for (dst, off) in ((tmpa, 0.5), (ang, 0.75)):
                rot_fns += [
                    lambda e, dst=dst, off=off: e.tensor_scalar(out=dst[:], in0=ang[:], scalar1=1.0 / (2 * PI), scalar2=off, op0=ALU.mult, op1=ALU.add),
                    lambda e, dst=dst: e.tensor_copy(out=posi[:], in_=dst[:]),
                    lambda e: e.tensor_copy(out=tmpb[:], in_=posi[:]),
                    lambda e, dst=dst: e.tensor_tensor(out=dst[:], in0=dst[:], in1=tmpb[:], op=ALU.subtract),
                    lambda e, dst=dst: e.tensor_scalar(out=tmpb[:], in0=dst[:], scalar1=0.0, scalar2=None, op0=ALU.is_lt),
                    lambda e, dst=dst: e.tensor_tensor(out=dst[:], in0=dst[:], in1=tmpb[:], op=ALU.add)]
            S.seq("dve", rot_fns, reads=[b_rot, b_rc], writes=[b_rot])

            def rot2(e):
                e.activation(out=St[:], in_=tmpa[:], func=AF.Sin, bias=negpi[:, 0:1], scale=2 * PI)
                return e.activation(out=Ct[:], in_=ang[:], func=AF.Sin, bias=negpi[:, 0:1], scale=2 * PI)
            S.act(rot2, reads=[b_rot], writes=[b_rot])
            S.dve(lambda e: e.tensor_scalar(out=St[:], in0=St[:], scalar1=rc[:, 1:2], scalar2=None, op0=ALU.mult), reads=[b_rot, b_rc], writes=[b_rot])

            S.flush()
            st1x.close()
            if stop_after <= 1:
                return nc, S
            sb, ps = sb_o, ps_o
            b_rot = S.buf("rot2")
            nw1 = sb("nw1", [128, 8], F32)
            b_nw1 = S.buf("nw1")
            S.dma("sp", lambda e: e.dma_start(out=nw1[:], in_=normw1), writes=[b_nw1])
            wf = [sb(f"wf{i}", [128, 8, 512], F32) for i in range(2)]
            b_wf = S.bufs(2, "wf")
            wb = [sb(f"wb{i}", [128, 8, 512], BF16) for i in range(2)]
            b_wb = S.bufs(2, "wb")
            ostg = [sb(f"ostg{i}", [128, TOK], BF16) for i in range(2)]
            b_ostg = S.bufs(2, "ostg")
            t1 = [sb(f"t1_{i}", [128, 512], F32) for i in range(2)]
            b_t1 = S.bufs(2, "t1")
            t2 = [sb(f"t2_{i}", [128, 512], F32) for i in range(2)]
            b_t2 = S.bufs(2, "t2")
            tstg = [sb(f"tstg{i}", [128, 512], BF16) for i in range(3)]
            b_tstg = S.bufs(3, "tstg")
            tstf = [sb(f"tstf{i}", [128, 32], F32) for i in range(2)]
            b_tstf = S.bufs(2, "tstf")
            pf = [ps(f"pf{i}", [128, 512], F32) for i in range(4)]
            b_pf = S.bufs(4, "pf")
            w_v = w_in.rearrange("(kc p) n -> p kc n", p=128)
            all_hT = b_hT

            def load_w(g, ncols=512):
                u = g % 2
                c0 = g * 512
                S.dma("sp", lambda e: e.dma_start(out=wf[u][:, :, 0:ncols], in_=w_v[:, :, c0:c0 + ncols]), writes=[b_wf[u]])
                S.pool(lambda e: e.tensor_tensor(out=wb[u][:, :, 0:ncols], in0=wf[u][:, :, 0:ncols], in1=bc3(nw1[:, :], ncols), op=ALU.mult),
                       reads=[b_wf[u], b_nw1], writes=[b_wb[u]])
                return u

            pfi = [0]

            def mm_feat(u, c, tg):
                k = pfi[0] % 4
                pfi[0] += 1

                def f(e):
                    r = None
                    for kc in range(8):
                        r = e.matmul(pf[k][:], lhsT=wb[u][:, kc, c * 128:(c + 1) * 128], rhs=hT[:, kc, tg * 512:(tg + 1) * 512],
                                     start=(kc == 0), stop=(kc == 7))
                    return r
                S.pe(f, reads=[b_wb[u]] + all_hT[tg * 4:(tg + 1) * 4], writes=[b_pf[k]])
                return k

            evi = [0]
            for g in range(16):
                u = load_w(g)
                if g < 8:
                    for pair in range(2):
                        head = (g % 4) * 2 + pair
                        so = (g * 2 + pair) % 2
                        for tg in range(8):
                            ka = mm_feat(u, pair * 2, tg)
                            kb = mm_feat(u, pair * 2 + 1, tg)
                            tt = tg % 2
                            S.dve(lambda e, ka=ka, tg=tg, tt=tt: e.tensor_tensor(out=t1[tt][:], in0=pf[ka][:], in1=Ct[:, tg * 512:(tg + 1) * 512], op=ALU.mult),
                                  reads=[b_pf[ka], b_rot], writes=[b_t1[tt]])
                            S.dve(lambda e, kb=kb, tg=tg, tt=tt: e.tensor_tensor(out=t2[tt][:], in0=pf[kb][:], in1=St[:, tg * 512:(tg + 1) * 512], op=ALU.mult),
                                  reads=[b_pf[kb], b_rot], writes=[b_t2[tt]])
                            S.pool(lambda e, so=so, tg=tg, tt=tt: e.tensor_tensor(out=ostg[so][:, tg * 512:(tg + 1) * 512], in0=t1[tt][:], in1=t2[tt][:], op=ALU.add),
                                   reads=[b_t1[tt], b_t2[tt]], writes=[b_ostg[so]])
                        dst = QT if g < 4 else KT
                        S.dma("sp", lambda e, dst=dst, head=head, so=so: e.dma_start(out=dst[head], in_=ostg[so][:]), reads=[b_ostg[so]], writes=[S.dout("qk")], waw=False)
                else:
                    for c in range(4):
                        cc = (g - 8) * 4 + c
                        so = cc % 2
                        for tg in range(8):
                            k = mm_feat(u, c, tg)
                            if evi[0] % 2 == 0:
                                S.act(lambda e, k=k, so=so, tg=tg: e.copy(out=ostg[so][:, tg * 512:(tg + 1) * 512], in_=pf[k][:]), reads=[b_pf[k]], writes=[b_ostg[so]])
                            else:
                                S.dve(lambda e, k=k, so=so, tg=tg: e.tensor_copy(out=ostg[so][:, tg * 512:(tg + 1) * 512], in_=pf[k][:]), reads=[b_pf[k]], writes=[b_ostg[so]])
                            evi[0] += 1
                        S.dma("sp", lambda e, cc=cc, so=so: e.dma_start(out=XBCT[cc], in_=ostg[so][:]), reads=[b_ostg[so]], writes=[S.dout("xbc")], waw=False)
            tmaj = [(VT, 0, "copy"), (VT, 512, "copy"), (ZS, 0, "silu"), (ZS, 512, "silu"), (ZS, 1024, "silu"), (ZS, 1536, "silu"),
                    (GA, 0, "sig"), (GA, 512, "sig"), (GS, 0, "sig"), (GS, 512, "sig")]
            tsi = [0]
            for gi, (dst, c0, kind) in enumerate(tmaj):
                g = 16 + gi
                u = load_w(g)
                for i in range(NT):
                    k = pfi[0] % 4
                    pfi[0] += 1

                    def f(e, k=k, i=i, u=u):
                        r = None
                        for kc in range(8):
                            r = e.matmul(pf[k][:], lhsT=hT[:, kc, i * 128:(i + 1) * 128], rhs=wb[u][:, kc, :], start=(kc == 0), stop=(kc == 7))
                        return r
                    S.pe(f, reads=[b_wb[u], b_hT[i]], writes=[b_pf[k]])
                    ts_ = tsi[0] % 3
                    tsi[0] += 1
                    if kind == "copy":
                        S.dve(lambda e, k=k, ts_=ts_: e.tensor_copy(out=tstg[ts_][:], in_=pf[k][:]), reads=[b_pf[k]], writes=[b_tstg[ts_]])
                    else:
                        fn = AF.Silu if kind == "silu" else AF.Sigmoid
                        S.act(lambda e, k=k, ts_=ts_, fn=fn: e.activation(out=tstg[ts_][:], in_=pf[k][:], func=fn), reads=[b_pf[k]], writes=[b_tstg[ts_]])
                    S.dma("sp", lambda e, dst=dst, c0=c0, i=i, ts_=ts_: e.dma_start(out=dst[i * 128:(i + 1) * 128, c0:c0 + 512], in_=tstg[ts_][:]),
                          reads=[b_tstg[ts_]], writes=[S.dout("tm")], waw=False)
            u = load_w(26, ncols=32)
            for i in range(NT):
                k = pfi[0] % 4
                pfi[0] += 1

                def f(e, k=k, i=i, u=u):
                    r = None
                    for kc in range(8):
                        r = e.matmul(pf[k][:, 0:32], lhsT=hT[:, kc, i * 128:(i + 1) * 128], rhs=wb[u][:, kc, 0:32], start=(kc == 0), stop=(kc == 7))
                    return r
                S.pe(f, reads=[b_wb[u], b_hT[i]], writes=[b_pf[k]])
                ts_ = i % 2
                S.dve(lambda e, k=k, ts_=ts_: e.tensor_copy(out=tstf[ts_][:], in_=pf[k][:, 0:32]), reads=[b_pf[k]], writes=[b_tstf[ts_]])
                S.dma("sp", lambda e, i=i, ts_=ts_: e.dma_start(out=DTR[i * 128:(i + 1) * 128, :], in_=tstf[ts_][:]), reads=[b_tstf[ts_]], writes=[S.dout("dtr")], waw=False)
            S.flush()
        if stop_after <= 2:
            return nc, S

        lam_init = 0.8 - 0.6 * math.exp(-0.3 * 0)
        with contextlib.ExitStack() as st:
            def sb(name, shape, dt):
                return st.enter_context(nc.sbuf_tensor(name, list(shape), dt))

            def ps(name, shape, dt):
                return st.enter_context(nc.psum_tensor(name, list(shape), dt))

            lamv = sb("lamv", [128, 256], F32)
            lamw = sb("lamw", [128, 8], F32)
            lamj = sb("lamj", [128, 64], F32)
            sublnb = sb("sublnb", [128, 128], F32)
            b_lam = S.buf("lam")
            b_sub = S.buf("subln")
            S.dma("sp", lambda e: e.dma_start(out=lamv[:], in_=lam4.partition_broadcast(128)), writes=[b_lam])
            S.dma("sp", lambda e: e.dma_start(out=sublnb[:], in_=subln.partition_broadcast(128)), writes=[b_sub])

            S.seq("dve", [
                lambda e: e.tensor_tensor(out=lamj[:], in0=lamv[:, 0:64], in1=lamv[:, 64:128], op=ALU.mult),
                lambda e: e.tensor_reduce(out=lamw[:, 0:1], in_=lamj[:], axis=AX.X, op=ALU.add),
                lambda e: e.tensor_tensor(out=lamj[:], in0=lamv[:, 128:192], in1=lamv[:, 192:256], op=ALU.mult),
                lambda e: e.tensor_reduce(out=lamw[:, 1:2], in_=lamj[:], axis=AX.X, op=ALU.add),
            ], reads=[b_lam], writes=[b_lam])
            S.act(lambda e: e.activation(out=lamw[:, 2:4], in_=lamw[:, 0:2], func=AF.Exp), reads=[b_lam], writes=[b_lam])

            S.seq("dve", [
                lambda e: e.tensor_tensor(out=lamw[:, 4:5], in0=lamw[:, 3:4], in1=lamw[:, 2:3], op=ALU.subtract),
                lambda e: e.tensor_scalar(out=lamw[:, 5:6], in0=lamw[:, 4:5], scalar1=-lam_init, scalar2=None, op0=ALU.add),
            ], reads=[b_lam], writes=[b_lam])
            S.dve(lambda e: e.tensor_scalar(out=sublnb[:], in0=sublnb[:], scalar1=1.0 - lam_init, scalar2=None, op0=ALU.mult), reads=[b_sub], writes=[b_sub])

            qt = [sb(f"qt{i}", [128, 2048], BF16) for i in range(2)]
            kt = [sb(f"kt{i}", [128, 2048], BF16) for i in range(2)]
            vt = [sb(f"vt{i}", [128, 16, 130], BF16) for i in range(2)]
            b_qt = S.bufs(2, "qt")
            b_kt = S.bufs(2, "kt")
            b_vt = S.bufs(2, "vt")
            pT = [sb(f"pT{i}", [128, 512], BF16) for i in range(3)]
            b_pT = S.bufs(3, "pT")
            oc = [sb(f"oc{i}", [128, 16, 128], F32) for i in range(2)]
            b_oc = S.bufs(2, "oc")
            rcp = sb("rcp", [128, 64], F32)
            b_rcp = S.bufs(4, "rcp")
            dd = sb("dd", [128, 16, 128], F32)
            sq = sb("sq", [128, 16, 128], F32)
            sst = sb("sst", [128, 64], F32)
            b_dd = S.buf("dd")
            attb = [sb(f"attb{i}", [128, 16, 128], BF16) for i in range(2)]
            b_attb = S.bufs(2, "attb")
            pS = [ps(f"pS{i}", [128, 512], F32) for i in range(3)]
            b_pS = S.bufs(3, "pS")
            pO = [ps(f"pO{i}", [128, 512], F32) for i in range(4)]
            b_pO = S.bufs(4, "pO")
            for u in range(2):
                S.pool(lambda e, u=u: e.memset(vt[u][:, :, 128:129], 1.0), writes=[b_vt[u]])
            zfill = sb("zfill", [128, 8192], F32)
            b_zf = S.buf("zfill")
            S.pool(lambda e: e.memset(zfill[:], 0.0), writes=[b_zf])
            for zi in range(16):
                S.dma("sp", lambda e, zi=zi: e.dma_start(out=XBUF[zi * 1024:(zi + 1) * 1024, :].rearrange("(p j) d -> p (j d)", p=128), in_=zfill[:]),
                      reads=[b_zf], writes=[S.dout("xbuf0")], waw=False)
            si = [0]
            for s in range(2):
                for h in range(8):
                    u = (s * 8 + h) % 2
                    t0 = s * 2048
                    S.dma("sp", lambda e, u=u, h=h, t0=t0: e.dma_start(out=qt[u][:], in_=QT[h, :, t0:t0 + 2048]), writes=[b_qt[u]])
                    S.dma("sp", lambda e, u=u, h=h, t0=t0: e.dma_start(out=kt[u][:], in_=KT[h, :, t0:t0 + 2048]), writes=[b_kt[u]])
                    S.dma("sp", lambda e, u=u, h=h, t0=t0: e.dma_start(out=vt[u][:, :, 0:128],
                                                                          in_=VT[t0:t0 + 2048, h * 128:(h + 1) * 128].rearrange("(j p) e -> p j e", p=128)),
                          writes=[b_vt[u]])
                    for c in range(2):
                        for qc in range(4):
                            q_hi = (4 * qc + 4) * 128

                            def emit_S(j, u=u, c=c, qc=qc, q_hi=q_hi):
                                q_lo = max(j, 4 * qc) * 128
                                wd = q_hi - q_lo
                                r = si[0] % 3
                                si[0] += 1
                                S.pe(lambda e, r=r, u=u, c=c, j=j, q_lo=q_lo, q_hi=q_hi, wd=wd: e.matmul(
                                    pS[r][:, 0:wd], lhsT=kt[u][c * 64:(c + 1) * 64, j * 128:(j + 1) * 128],
                                    rhs=qt[u][c * 64:(c + 1) * 64, q_lo:q_hi], start=True, stop=True),
                                    reads=[b_kt[u], b_qt[u]], writes=[b_pS[r]])
                                return r
                            nj = 4 * qc + 4
                            r_next = emit_S(0)
                            for j in range(nj):
                                qb_lo = max(j, 4 * qc)
                                q_lo = qb_lo * 128
                                wd = q_hi - q_lo
                                r = r_next
                                if j + 1 < nj:
                                    r_next = emit_S(j + 1)
                                S.act(lambda e, r=r, wd=wd: e.activation(out=pT[r][:, 0:wd], in_=pS[r][:, 0:wd], func=AF.Exp, scale=0.125),
                                      reads=[b_pS[r]], writes=[b_pT[r]])
                                if j >= 4 * qc:
                                    S.pool(lambda e, r=r: e.affine_select(out=pT[r][:, 0:128], in_=pT[r][:, 0:128], pattern=[[1, 128]],
                                                                          compare_op=ALU.is_ge, fill=0.0, base=0, channel_multiplier=-1),
                                           reads=[b_pT[r]], writes=[b_pT[r]])
                                for i in range(qb_lo, 4 * qc + 4):
                                    ob = i - 4 * qc
                                    S.pe(lambda e, r=r, u=u, i=i, j=j, ob=ob, q_lo=q_lo: e.matmul(
                                        pO[ob][:, 0:129], lhsT=pT[r][:, i * 128 - q_lo:i * 128 - q_lo + 128], rhs=vt[u][:, j, 0:129],
                                        start=(j == 0), stop=(j == i)), reads=[b_pT[r], b_vt[u]], writes=[b_pO[ob]])
                            for ob in range(4):
                                i = 4 * qc + ob
                                S.dve(lambda e, ob=ob, i=i: e.reciprocal(out=rcp[:, ob * 16 + i:ob * 16 + i + 1], in_=pO[ob][:, 128:129]),
                                      reads=[b_pO[ob]], writes=[b_rcp[ob]])
                                S.dve(lambda e, ob=ob, i=i, c=c: e.tensor_scalar(out=oc[c][:, i, :], in0=pO[ob][:, 0:128],
                                                                                  scalar1=rcp[:, ob * 16 + i:ob * 16 + i + 1], scalar2=None, op0=ALU.mult),
                                      reads=[b_pO[ob], b_rcp[ob]], writes=[b_oc[c]])
                    S.dve(lambda e: e.scalar_tensor_tensor(out=dd[:], in0=oc[1][:], scalar=lamw[:, 5:6], in1=oc[0][:], op0=ALU.mult, op1=ALU.add),
                          reads=[b_oc[0], b_oc[1], b_lam], writes=[b_dd])
                    S.pool(lambda e: e.tensor_tensor(out=sq[:], in0=dd[:], in1=dd[:], op=ALU.mult), reads=[b_dd], writes=[b_dd])

                    S.seq("dve", [
                        lambda e: e.tensor_reduce(out=sst[:, 0:16], in_=sq[:], axis=AX.X, op=ALU.add),
                        lambda e: e.tensor_scalar(out=sst[:, 16:32], in0=sst[:, 0:16], scalar1=1.0 / 128, scalar2=1e-5, op0=ALU.mult, op1=ALU.add),
                    ], reads=[b_dd], writes=[b_dd])
                    S.act(lambda e: e.activation(out=sst[:, 32:48], in_=sst[:, 16:32], func=AF.Sqrt), reads=[b_dd], writes=[b_dd])
                    S.dve(lambda e: e.reciprocal(out=sst[:, 48:64], in_=sst[:, 32:48]), reads=[b_dd], writes=[b_dd])
                    S.dve(lambda e: e.tensor_tensor(out=dd[:], in0=dd[:], in1=bc3(sst[:, 48:64], 128), op=ALU.mult), reads=[b_dd], writes=[b_dd])
                    S.pool(lambda e, u=u: e.tensor_tensor(out=attb[u][:], in0=dd[:], in1=bcmid(sublnb[:, :], 16), op=ALU.mult),
                           reads=[b_dd, b_sub], writes=[b_attb[u]])
                    S.dma("sp", lambda e, u=u, h=h, t0=t0: e.dma_start(
                        out=ATT[t0:t0 + 2048, h * 128:(h + 1) * 128].rearrange("(j p) e -> p j e", p=128), in_=attb[u][:]),
                        reads=[b_attb[u]], writes=[S.dout("att")], waw=False)
            S.flush()
        if stop_after <= 3:
            return nc, S

        with contextlib.ExitStack() as st:
            def sb(name, shape, dt):
                return st.enter_context(nc.sbuf_tensor(name, list(shape), dt))

            def ps(name, shape, dt):
                return st.enter_context(nc.psum_tensor(name, list(shape), dt))

            cw = sb("cw", [128, 32, 4], F32)
            cb = sb("cb", [128, 32], F32)
            diag = sb("diag", [128, 32, 4, 128], BF16)
            b_cw = S.buf("cw")
            b_diag = S.buf("diag")
            vec3 = sb("vec3", [128, 96], F32)
            Abc = sb("Abc", [128, 32], F32)
            b_vec = S.buf("vec3")
            snw = sb("snw", [128, 2048], F32)
            b_snw = S.buf("snw")
            S.dma("sp", lambda e: e.dma_start(out=cw[:], in_=convw), writes=[b_cw])
            b_cb = S.buf("cb")
            S.dma("sp", lambda e: e.dma_start(out=cb[:], in_=convb), writes=[b_cb])
            S.dma("sp", lambda e: e.dma_start(out=vec3[:, 0:32], in_=dt_bias.partition_broadcast(128)), writes=[b_vec], waw=False)
            S.dma("sp", lambda e: e.dma_start(out=vec3[:, 32:64], in_=a_log.partition_broadcast(128)), writes=[b_vec], waw=False)
            S.dma("sp", lambda e: e.dma_start(out=vec3[:, 64:96], in_=d_skip.partition_broadcast(128)), writes=[b_vec], waw=False)
            S.dma("sp", lambda e: e.dma_start(out=snw[:], in_=ssd_nw.partition_broadcast(128)), writes=[b_snw])

            def mkdiag(e):
                r = None
                for cc in range(32):
                    for k in range(4):
                        r = e.tensor_scalar(out=diag[:, cc, k, :], in0=ident_f[:], scalar1=cw[:, cc, k:k + 1], scalar2=None, op0=ALU.mult)
                return r
            S.pool(mkdiag, reads=[b_cw], writes=[b_diag])
            S.act(lambda e: e.activation(out=Abc[:], in_=vec3[:, 32:64], func=AF.Exp), reads=[b_vec], writes=[b_vec])
            S.dve(lambda e: e.tensor_scalar(out=Abc[:], in0=Abc[:], scalar1=-1.0, scalar2=None, op0=ALU.mult), reads=[b_vec], writes=[b_vec])

            xr = [sb(f"xr{i}", [128, 32, 132], BF16) for i in range(2)]
            b_xrh = S.bufs(2, "xrh")
            b_xrA = S.bufs(2, "xrA")
            b_xrB = S.bufs(2, "xrB")
            import os as _os
            P4L = int(_os.environ.get("P4L", "99"))
            xc = [sb(f"xc{i}", [128, 32, 128], BF16) for i in range(2)]
            b_xc = S.bufs(2, "xc")
            xs_f_l = [sb(f"xs_f{i}", [128, 2048], F32) for i in range(2)]
            ztf = sb("ztf", [128, 2048], F32)
            b_ztf = S.buf("ztf")
            xdt_l = [sb(f"xdt{i}", [128, 2048], BF16) for i in range(2)]
            xdtw_l = [sb(f"xdtw{i}", [128, 2048], BF16) for i in range(2)]
            B_tok_l = [sb(f"B_tok{i}", [128, 8, 128], BF16) for i in range(2)]
            b_xs_l = S.bufs(2, "xs_f")
            b_xdt_l = S.bufs(2, "xdt")
            b_xdtw_l = S.bufs(2, "xdtw")
            b_Bt_l = S.bufs(2, "B_tok")
            dtt_l = [sb(f"dtt{i}", [128, 8, 32], F32) for i in range(2)]
            dtt2_l = [sb(f"dtt2{i}", [128, 3, 32], F32) for i in range(2)]
            b_dt_l = S.bufs(2, "dtt")
            E = [sb(f"E{i}", [128, 4, 128], F32) for i in range(2)]
            b_E = S.bufs(2, "E")
            M = [sb(f"M{i}", [128, 4, 128], BF16) for i in range(2)]
            b_M = S.bufs(2, "M")
            yoff = [sb(f"yoff{i}", [128, 256], F32) for i in range(2)]
            b_yoff = S.bufs(2, "yoff")
            y_l = [sb(f"y{i}", [128, 2048], F32) for i in range(2)]
            b_y_l = S.bufs(2, "y")
            ytmp_l = [sb(f"ytmp{i}", [128, 2048], F32) for i in range(2)]
            b_ytmp_l = S.bufs(2, "ytmp")
            zt = [sb(f"zt{i}", [128, 2048], BF16) for i in range(2)]
            b_zt = S.bufs(2, "zt")
            yo = [sb(f"yo{i}", [128, 2048], BF16) for i in range(2)]
            b_yo = S.bufs(2, "yo")
            gst_ = sb("gst", [128, 32], F32)
            Sst = sb("Sst", [128, 8, 256], F32)
            Sbf = sb("Sbf", [128, 8, 256], BF16)
            b_S = S.bufs(8, "Sst")
            b_Sbf = S.bufs(8, "Sbf")
            pcv = [ps(f"pcv{i}", [128, 4, 128], F32) for i in range(2)]
            b_pcv = S.bufs(2, "pcv")
            ptx = ps("ptx", [128, 16, 128], BF16)
            b_ptx = S.buf("ptx")
            pmisc = ps("pmisc", [128, 512], F32)
            b_pcm = S.buf("pcm")
            b_pcb = b_pcm
            b_pyo = b_pcm
            pseg = [ps(f"pseg{i}", [128, 4, 128], F32) for i in range(2)]
            b_pseg = S.bufs(2, "pseg")
            py = ps("py", [128, 512], F32)
            b_py = S.buf("py")
            b_pst = b_py
            ev = [0]
            pending = [None]
            for s in range(2 if P4L >= 99 else 1):
                for ch in range(16 if P4L >= 99 else 2):
                    def tile_body(s=s, ch=ch):
                        ti = s * 16 + ch
                        u = ti % 2
                        xs_f, xdt, xdtw, B_tok, dtt, dtt2, y, ytmp = xs_f_l[u], xdt_l[u], xdtw_l[u], B_tok_l[u], dtt_l[u], dtt2_l[u], y_l[u], ytmp_l[u]
                        b_xs, b_xdt, b_xdtw, b_Bt, b_dt, b_y, b_ytmp = b_xs_l[u], b_xdt_l[u], b_xdtw_l[u], b_Bt_l[u], b_dt_l[u], b_y_l[u], b_ytmp_l[u]
                        ti = s * 16 + ch
                        t0 = ti * 128
                        u = ti % 2
                        if ch == 0:
                            S.pool(lambda e, u=u: e.memset(xr[u][:, :, 0:4], 0.0), writes=[b_xrh[u]])
                            S.pool(lambda e: e.memset(Sst[:], 0.0), writes=b_S)
                            S.pool(lambda e: e.memset(Sbf[:], 0.0), writes=b_Sbf)
                        else:
                            S.pool(lambda e, u=u: e.tensor_copy(out=xr[u][:, :, 1:4], in_=xr[1 - u][:, :, 129:132]),
                                   reads=[b_xrA[1 - u], b_xrB[1 - u]], writes=[b_xrh[u]])
                        S.dma("sp", lambda e, u=u, t0=t0: e.dma_start(out=xr[u][:, 0:16, 4:132], in_=XBCT[0:16, :, t0:t0 + 128].rearrange("c p t -> p c t")),
                              writes=[b_xrA[u]])
                        S.dma("sp", lambda e, u=u, t0=t0: e.dma_start(out=xr[u][:, 16:32, 4:132], in_=XBCT[16:32, :, t0:t0 + 128].rearrange("c p t -> p c t")),
                              writes=[b_xrB[u]])
                        S.dma("sp", lambda e, t0=t0: e.dma_start(out=dtt[:, 0, :], in_=DTR[t0:t0 + 128, :]), writes=[b_dt])
                        S.dma("sp", lambda e, u=u, t0=t0: e.dma_start(out=zt[u][:], in_=ZS[t0:t0 + 128, :]), writes=[b_zt[u]])
                        if P4L <= 1:
                            return
                        S.dve(lambda e: e.tensor_tensor(out=dtt[:, 1, :], in0=dtt[:, 0, :], in1=vec3[:, 0:32], op=ALU.add), reads=[b_dt, b_vec], writes=[b_dt])
                        S.act(lambda e: e.activation(out=dtt[:, 2, :], in_=dtt[:, 1, :], func=AF.Exp), reads=[b_dt], writes=[b_dt])
                        S.act(lambda e: e.activation(out=dtt[:, 3, :], in_=dtt[:, 2, :], func=AF.Ln, bias=1.0, scale=1.0), reads=[b_dt], writes=[b_dt])
                        S.dve(lambda e: e.tensor_tensor(out=dtt[:, 4, :], in0=dtt[:, 3, :], in1=Abc[:], op=ALU.mult), reads=[b_dt, b_vec], writes=[b_dt])

                        def cumf(e):
                            e.matmul(pmisc[:, 0:32], lhsT=tri_f[:], rhs=dtt[:, 4, :], start=True, stop=True)
                            return e.matmul(pmisc[:, 32:64], lhsT=ones_f[:], rhs=dtt[:, 4, :], start=True, stop=True)
                        S.pe(cumf, reads=[b_dt], writes=[b_pcm])
                        S.dve(lambda e: e.tensor_scalar(out=dtt[:, 5, :], in0=pmisc[:, 0:32], scalar1=-1.0, scalar2=None, op0=ALU.mult), reads=[b_pcm], writes=[b_dt])
                        S.dve(lambda e: e.tensor_tensor(out=dtt2[:, 1, :], in0=pmisc[:, 32:64], in1=dtt[:, 5, :], op=ALU.add), reads=[b_pcm, b_dt], writes=[b_dt])

                        def expf(e):
                            e.activation(out=dtt[:, 6, :], in_=pmisc[:, 0:32], func=AF.Exp)
                            e.activation(out=dtt2[:, 0, :], in_=pmisc[:, 32:64], func=AF.Exp)
                            return e.activation(out=dtt[:, 7, :], in_=dtt2[:, 1, :], func=AF.Exp)
                        S.act(expf, reads=[b_pcm, b_dt], writes=[b_dt])
                        if P4L <= 2:
                            return
                        for c4 in range(8):
                            pv = c4 % 2

                            def convf(e, c4=c4, pv=pv, u=u):
                                r = None
                                for q in range(4):
                                    cc = c4 * 4 + q
                                    for k in range(4):
                                        r = e.matmul(pcv[pv][:, q, :], lhsT=diag[:, cc, k, :], rhs=xr[u][:, cc, k + 1:k + 129], start=(k == 0), stop=(k == 3))
                                return r
                            S.pe(convf, reads=[b_diag, b_xrh[u], b_xrA[u], b_xrB[u]], writes=[b_pcv[pv]])

                            def siluf(e, c4=c4, pv=pv, u=u):
                                r = None
                                for q in range(4):
                                    cc = c4 * 4 + q
                                    r = e.activation(out=xc[u][:, cc, :], in_=pcv[pv][:, q, :], func=AF.Silu, bias=cb[:, cc:cc + 1], scale=1.0)
                                return r
                            S.act(siluf, reads=[b_pcv[pv], b_cb], writes=[b_xc[u]])
                        if P4L <= 3:
                            return

                        def tpf(e, u=u):
                            r = None
                            for cc in range(16):
                                r = e.transpose(out=ptx[:, cc, :], in_=xc[u][:, cc, :], identity=ident_b[:])
                            return r
                        S.pe(tpf, reads=[b_xc[u]], writes=[b_ptx])
                        def xscp(e):
                            e.copy(out=xs_f[:, 0:1024].rearrange("p (c t) -> p c t", t=128), in_=ptx[:, 0:8, :])
                            return e.copy(out=xs_f[:, 1024:2048].rearrange("p (c t) -> p c t", t=128), in_=ptx[:, 8:16, :])
                        S.act(xscp, reads=[b_ptx], writes=[b_xs])

                        def tpb(e, u=u):
                            r = None
                            for cc in range(8):
                                r = e.transpose(out=ptx[:, cc, :], in_=xc[u][:, 16 + cc, :], identity=ident_b[:])
                            return r
                        S.pe(tpb, reads=[b_xc[u]], writes=[b_ptx])
                        S.act(lambda e: e.copy(out=B_tok[:], in_=ptx[:, 0:8, :]), reads=[b_ptx], writes=[b_Bt])
                        S.dve(lambda e: e.tensor_tensor(out=dtt2[:, 2, :], in0=dtt[:, 3, :], in1=dtt[:, 7, :], op=ALU.mult), reads=[b_dt], writes=[b_dt])
                        S.dve(lambda e: e.tensor_tensor(out=xdt[:].rearrange("p (h d) -> p h d", d=64), in0=xs_f[:].rearrange("p (h d) -> p h d", d=64),
                                                        in1=bc3(dtt[:, 3, :], 64), op=ALU.mult), reads=[b_xs, b_dt], writes=[b_xdt])
                        S.pool(lambda e: e.tensor_tensor(out=xdtw[:].rearrange("p (h d) -> p h d", d=64), in0=xs_f[:].rearrange("p (h d) -> p h d", d=64),
                                                         in1=bc3(dtt2[:, 2, :], 64), op=ALU.mult), reads=[b_xs, b_dt], writes=[b_xdtw])
                        if P4L <= 4:
                            return
                        def emit_seg(g, ti=ti):
                            eu = (ti * 8 + g) % 2

                            def segf(e, g=g, eu=eu):
                                r = None
                                for r_ in range(4):
                                    hh = 4 * g + r_
                                    e.matmul(pseg[eu][:, r_, :], lhsT=dtt[:, 4, hh:hh + 1].to_broadcast([128, 128]), rhs=tri_f[:], start=True, stop=False)
                                    r = e.matmul(pseg[eu][:, r_, :], lhsT=ident_f[:], rhs=negmask[:], start=False, stop=True)
                                return r
                            S.pe(segf, reads=[b_dt], writes=[b_pseg[eu]])

                            def expg(e, g=g, eu=eu):
                                r = None
                                for r_ in range(4):
                                    hh = 4 * g + r_
                                    r = e.activation(out=E[eu][:, r_, :], in_=pseg[eu][:, r_, :], func=AF.Exp, bias=dtt[:, 5, hh:hh + 1], scale=1.0)
                                return r
                            S.act(expg, reads=[b_pseg[eu], b_dt], writes=[b_E[eu]])
                        emit_seg(0)
                        for g in range(8):
                            eu = (ti * 8 + g) % 2
                            S.pe(lambda e, g=g, u=u: e.matmul(pmisc[:, 128:256], lhsT=xc[u][:, 16 + g, :], rhs=xc[u][:, 24 + g, :], start=True, stop=True),
                                 reads=[b_xc[u]], writes=[b_pcb])
                            S.dve(lambda e, eu=eu: e.tensor_tensor(out=M[eu][:], in0=E[eu][:], in1=bcmid(pmisc[:, 128:256], 4), op=ALU.mult),
                                  reads=[b_E[eu], b_pcb], writes=[b_M[eu]])
                            if g + 1 < 8:
                                emit_seg(g + 1)

                            def ydf(e, g=g, eu=eu):
                                r = None
                                for r_ in range(4):
                                    hh = 4 * g + r_
                                    r = e.matmul(py[:, r_ * 64:(r_ + 1) * 64], lhsT=M[eu][:, r_, :], rhs=xdt[:, hh * 64:(hh + 1) * 64], start=True, stop=True)
                                return r
                            S.pe(ydf, reads=[b_M[eu], b_xdt], writes=[b_py])
                            S.pe(lambda e, g=g, u=u: e.matmul(pmisc[:, 256:512], lhsT=xc[u][:, 24 + g, :], rhs=Sbf[:, g, :], start=True, stop=True),
                                 reads=[b_xc[u], b_Sbf[g]], writes=[b_pyo])

                            def yoffs(e, g=g, eu=eu):
                                r = None
                                for r_ in range(4):
                                    hh = 4 * g + r_
                                    r = e.activation(out=yoff[eu][:, r_ * 64:(r_ + 1) * 64], in_=pmisc[:, 256 + r_ * 64:256 + (r_ + 1) * 64], func=AF.Copy,
                                                     scale=dtt[:, 6, hh:hh + 1])
                                return r
                            S.act(yoffs, reads=[b_pyo, b_dt], writes=[b_yoff[eu]])
                            S.dve(lambda e, g=g, eu=eu: e.tensor_tensor(out=y[:, g * 256:(g + 1) * 256], in0=py[:, 0:256], in1=yoff[eu][:], op=ALU.add),
                                  reads=[b_py, b_yoff[eu]], writes=[b_y])
                            S.pe(lambda e, g=g: e.matmul(py[:, 256:512], lhsT=B_tok[:, g, :], rhs=xdtw[:, g * 256:(g + 1) * 256], start=True, stop=True),
                                 reads=[b_Bt, b_xdtw], writes=[b_pst])
                            S.pool(lambda e, g=g: e.tensor_tensor(out=Sst[:, g, :].rearrange("p (r d) -> p r d", d=64), in0=Sst[:, g, :].rearrange("p (r d) -> p r d", d=64),
                                                                  in1=bc3(dtt2[:, 0, 4 * g:4 * g + 4], 64), op=ALU.mult), reads=[b_dt, b_Sbf[g]], writes=[b_S[g]])
                            S.dve(lambda e, g=g: e.tensor_tensor(out=Sst[:, g, :], in0=py[:, 256:512], in1=Sst[:, g, :], op=ALU.add), reads=[b_pst, b_S[g]], writes=[b_S[g]])
                            S.pool(lambda e, g=g: e.tensor_copy(out=Sbf[:, g, :], in_=Sst[:, g, :]), reads=[b_S[g]], writes=[b_Sbf[g]])
                        if P4L <= 5:
                            return
                        yield
                        S.pool(lambda e, u=u: e.tensor_copy(out=ztf[:], in_=zt[u][:]), reads=[b_zt[u]], writes=[b_ztf])
                        S.pool(lambda e: e.tensor_tensor(out=ytmp[:].rearrange("p (h d) -> p h d", d=64), in0=xs_f[:].rearrange("p (h d) -> p h d", d=64),
                                                         in1=bc3(vec3[:, 64:96], 64), op=ALU.mult), reads=[b_xs, b_vec], writes=[b_ytmp])
                        S.dve(lambda e: e.tensor_tensor(out=y[:], in0=y[:], in1=ytmp[:], op=ALU.add), reads=[b_y, b_ytmp], writes=[b_y])
                        S.dve(lambda e: e.tensor_tensor(out=y[:], in0=y[:], in1=ztf[:], op=ALU.mult), reads=[b_y, b_ztf], writes=[b_y])
                        S.pool(lambda e: e.tensor_tensor(out=ytmp[:], in0=y[:], in1=y[:], op=ALU.mult), reads=[b_y], writes=[b_ytmp])

                        S.seq("dve", [
                            lambda e: e.tensor_reduce(out=gst_[:, 0:8], in_=ytmp[:].rearrange("p (g d) -> p g d", d=256), axis=AX.X, op=ALU.add),
                            lambda e: e.tensor_scalar(out=gst_[:, 8:16], in0=gst_[:, 0:8], scalar1=1.0 / 256, scalar2=1e-5, op0=ALU.mult, op1=ALU.add),
                        ], reads=[b_ytmp], writes=[b_ytmp])
                        S.act(lambda e: e.activation(out=gst_[:, 16:24], in_=gst_[:, 8:16], func=AF.Sqrt), reads=[b_ytmp], writes=[b_ytmp])
                        S.dve(lambda e: e.reciprocal(out=gst_[:, 24:32], in_=gst_[:, 16:24]), reads=[b_ytmp], writes=[b_ytmp])
                        S.dve(lambda e: e.tensor_tensor(out=y[:].rearrange("p (g d) -> p g d", d=256), in0=y[:].rearrange("p (g d) -> p g d", d=256),
                                                        in1=bc3(gst_[:, 24:32], 256), op=ALU.mult), reads=[b_y, b_ytmp], writes=[b_y])
                        S.pool(lambda e, u=u: e.tensor_tensor(out=yo[u][:], in0=y[:], in1=snw[:], op=ALU.mult), reads=[b_y, b_snw], writes=[b_yo[u]])
                        S.dma("sp", lambda e, u=u, t0=t0: e.dma_start(out=SSD[t0:t0 + 128, :], in_=yo[u][:]), reads=[b_yo[u]], writes=[S.dout("ssd")], waw=False)
                    gen = tile_body()
                    try:
                        next(gen)
                    except StopIteration:
                        gen = None
                    if pending[0] is not None:
                        for _ in pending[0]:
                            pass
                    pending[0] = gen
            if pending[0] is not None:
                for _ in pending[0]:
                    pass
            S.flush()
        if stop_after <= 4:
            return nc, S

        with contextlib.ExitStack() as st:
            def sb(name, shape, dt):
                return st.enter_context(nc.sbuf_tensor(name, list(shape), dt))

            def ps(name, shape, dt):
                return st.enter_context(nc.psum_tensor(name, list(shape), dt))

            LG = sb("LG", [128, NT, 72], F32)
            W1 = sb("W1", [128, NT], F32)
            W2 = sb("W2", [128, NT], F32)
            destI = sb("destI", [128, 2, NT], I32)
            IDX = sb("IDX", [128, 128], I32)
            b_LG = S.bufs(NT, "LG")
            with contextlib.ExitStack() as st5:
                def sb5(name, shape, dt):
                    return st5.enter_context(nc.sbuf_tensor(name, list(shape), dt))

                def ps5(name, shape, dt):
                    return st5.enter_context(nc.psum_tensor(name, list(shape), dt))
                Pa = sb5("Pa", [128, 8, 1024], BF16)
                Ps = sb5("Ps", [128, 16, 1024], BF16)
                Wo = sb5("Wo", [128, 8, 1024], BF16)
                Wr = sb5("Wr", [128, 8, 72], F32)
                brb = sb5("brb", [128, 72], F32)
                nw2 = sb5("nw2", [128, 1024], F32)
                b_W = S.buf("W5")
                wst = [sb5(f"wst{i}", [128, 8, 512], F32) for i in range(2)]
                b_wst = S.bufs(2, "wst")
                b_small = S.bufs(3, "small")
                S.dma("sp", lambda e: e.dma_start(out=Wr[:], in_=w_r.rearrange("(kc p) n -> p kc n", p=128)), writes=[b_small[0]])
                S.dma("sp", lambda e: e.dma_start(out=brb[:], in_=b_r.partition_broadcast(128)), writes=[b_small[1]])
                S.dma("sp", lambda e: e.dma_start(out=nw2[:], in_=normw2.partition_broadcast(128)), writes=[b_small[2]])
                wi = 0
                for (src, dstt, nk) in ((w_pa, Pa, 8), (w_ps, Ps, 16), (w_o, Wo, 8)):
                    sv = src.rearrange("(kc p) n -> p kc n", p=128)
                    for k0 in range(0, nk, 8):
                        for half in range(2):
                            u = wi % 2
                            wi += 1
                            S.dma("sp", lambda e, u=u, sv=sv, k0=k0, half=half: e.dma_start(out=wst[u][:], in_=sv[:, k0:k0 + 8, half * 512:(half + 1) * 512]),
                                  writes=[b_wst[u]])
                            S.pool(lambda e, u=u, dstt=dstt, k0=k0, half=half: e.tensor_copy(out=dstt[:, k0:k0 + 8, half * 512:(half + 1) * 512], in_=wst[u][:]),
                                   reads=[b_wst[u]], writes=[b_W])
                at = [sb5(f"at{i}", [128, 1024], BF16) for i in range(2)]
                sdt = [sb5(f"sdt{i}", [128, 2048], BF16) for i in range(2)]
                gat_ = [sb5(f"ga{i}", [128, 1024], BF16) for i in range(2)]
                gst2 = [sb5(f"gs{i}", [128, 1024], BF16) for i in range(2)]
                xt5 = [sb5(f"xt5_{i}", [128, 1024], F32) for i in range(2)]
                b_in5 = S.bufs(2, "in5")
                gaf = sb5("gaf", [128, 1024], F32)
                gsf = sb5("gsf", [128, 1024], F32)
                b_gf = S.buf("gf")
                aT = sb5("aT", [128, 8, 128], BF16)
                sT = sb5("sT", [128, 16, 128], BF16)
                mT = sb5("mT", [128, 8, 128], BF16)
                b_aT = S.buf("aT")
                b_sT = S.buf("sT")
                b_mT = S.buf("mT")
                m1 = sb5("m1", [128, 1024], F32)
                m2 = sb5("m2", [128, 1024], F32)
                mg = sb5("mg", [128, 1024], BF16)
                b_m1 = S.buf("m1")
                b_m2 = S.buf("m2")
                b_mg = S.buf("mg")
                x1t = [sb5(f"x1t{i}", [128, 1024], F32) for i in range(2)]
                b_x1t = S.bufs(2, "x1t")
                h2t = [sb5(f"h2t{i}", [128, 1024], F32) for i in range(2)]
                b_h2t = S.bufs(2, "h2t")
                h2T = sb5("h2T", [128, 8, 128], F32)
                b_h2T = S.buf("h2T")
                junk5 = sb5("junk5", [128, 1024], BF16)
                b_junk5 = S.buf("junk5")
                st5s = sb5("st5s", [128, NT, 4], F32)
                b_st5 = S.bufs(NT, "st5")
                pta = ps5("pta", [128, 8, 128], BF16)
                pts = ps5("pts", [128, 16, 128], BF16)
                b_pta = S.buf("pta")
                b_pts = S.buf("pts")
                pp = [ps5(f"pp{i}", [128, 512], F32) for i in range(3)]
                b_pp = S.bufs(3, "pp")
                ph = ps5("ph", [128, 8, 128], F32)
                b_ph = S.buf("ph")
                ppi = [0]

                def proj(lhs, bl, W, nk, half):
                    k = ppi[0] % 3
                    ppi[0] += 1

                    def f(e):
                        r = None
                        for kc in range(nk):
                            r = e.matmul(pp[k][:], lhsT=lhs[:, kc, :], rhs=W[:, kc, half * 512:(half + 1) * 512], start=(kc == 0), stop=(kc == nk - 1))
                        return r
                    S.pe(f, reads=[bl, b_W], writes=[b_pp[k]])
                    return k

                for i in range(NT):
                    u = i % 2
                    r0 = i * 128
                    S.dma("sp", lambda e, u=u, r0=r0: e.dma_start(out=at[u][:], in_=ATT[r0:r0 + 128, :]), writes=[b_in5[u]])
                    S.dma("sp", lambda e, u=u, r0=r0: e.dma_start(out=sdt[u][:], in_=SSD[r0:r0 + 128, :]), writes=[b_in5[u]], waw=False)
                    S.dma("sp", lambda e, u=u, r0=r0: e.dma_start(out=gat_[u][:], in_=GA[r0:r0 + 128, :]), writes=[b_in5[u]], waw=False)
                    S.dma("sp", lambda e, u=u, r0=r0: e.dma_start(out=gst2[u][:], in_=GS[r0:r0 + 128, :]), writes=[b_in5[u]], waw=False)
                    S.dma("sp", lambda e, u=u, r0=r0: e.dma_start(out=xt5[u][:], in_=x[r0:r0 + 128, :]), writes=[b_in5[u]], waw=False)

                    def tpa(e, u=u):
                        r = None
                        for kc in range(8):
                            r = e.transpose(out=pta[:, kc, :], in_=at[u][:, kc * 128:(kc + 1) * 128], identity=ident_b[:])
                        return r
                    S.pe(tpa, reads=[b_in5[u]], writes=[b_pta])
                    S.act(lambda e: e.copy(out=aT[:], in_=pta[:]), reads=[b_pta], writes=[b_aT])

                    def tps(e, u=u):
                        r = None
                        for kc in range(16):
                            r = e.transpose(out=pts[:, kc, :], in_=sdt[u][:, kc * 128:(kc + 1) * 128], identity=ident_b[:])
                        return r
                    S.pe(tps, reads=[b_in5[u]], writes=[b_pts])
                    def stcp(e):
                        e.copy(out=sT[:, 0:8, :], in_=pts[:, 0:8, :])
                        return e.copy(out=sT[:, 8:16, :], in_=pts[:, 8:16, :])
                    S.act(stcp, reads=[b_pts], writes=[b_sT])

                    def gcv(e, u=u):
                        e.tensor_copy(out=gaf[:], in_=gat_[u][:])
                        return e.tensor_copy(out=gsf[:], in_=gst2[u][:])
                    S.pool(gcv, reads=[b_in5[u]], writes=[b_gf])
                    for half in range(2):
                        hs = slice(half * 512, (half + 1) * 512)
                        ka = proj(aT, b_aT, Pa, 8, half)
                        S.dve(lambda e, ka=ka, hs=hs, u=u: e.tensor_tensor(out=m1[:, hs], in0=pp[ka][:], in1=gaf[:, hs], op=ALU.mult),
                              reads=[b_pp[ka], b_gf], writes=[b_m1])
                        ks = proj(sT, b_sT, Ps, 16, half)
                        S.dve(lambda e, ks=ks, hs=hs, u=u: e.tensor_tensor(out=m2[:, hs], in0=pp[ks][:], in1=gsf[:, hs], op=ALU.mult),
                              reads=[b_pp[ks], b_gf], writes=[b_m2])
                    S.pool(lambda e: e.tensor_tensor(out=mg[:], in0=m1[:], in1=m2[:], op=ALU.add), reads=[b_m1, b_m2], writes=[b_mg])

                    def tpm(e):
                        r = None
                        for kc in range(8):
                            r = e.transpose(out=pta[:, kc, :], in_=mg[:, kc * 128:(kc + 1) * 128], identity=ident_b[:])
                        return r
                    S.pe(tpm, reads=[b_mg], writes=[b_pta])
                    S.act(lambda e: e.copy(out=mT[:], in_=pta[:]), reads=[b_pta], writes=[b_mT])
                    for half in range(2):
                        hs = slice(half * 512, (half + 1) * 512)
                        ko = proj(mT, b_mT, Wo, 8, half)
                        S.dve(lambda e, ko=ko, hs=hs, u=u: e.tensor_tensor(out=x1t[u][:, hs], in0=pp[ko][:], in1=xt5[u][:, hs], op=ALU.add),
                              reads=[b_pp[ko], b_in5[u]], writes=[b_x1t[u]])
                    S.dma("sp", lambda e, u=u, r0=r0: e.dma_start(out=X1[r0:r0 + 128, :], in_=x1t[u][:]), reads=[b_x1t[u]], writes=[S.dout("x1")], waw=False)
                    S.act(lambda e, i=i, u=u: e.activation(out=junk5[:], in_=x1t[u][:], func=AF.Square, accum_out=st5s[:, i, 0:1]),
                          reads=[b_x1t[u]], writes=[b_junk5, b_st5[i]])
                    S.dve(lambda e, i=i: e.tensor_scalar(out=st5s[:, i, 1:2], in0=st5s[:, i, 0:1], scalar1=1.0 / D, scalar2=1e-6, op0=ALU.mult, op1=ALU.add),
                          reads=[b_st5[i]], writes=[b_st5[i]])
                    S.act(lambda e, i=i: e.activation(out=st5s[:, i, 2:3], in_=st5s[:, i, 1:2], func=AF.Sqrt), reads=[b_st5[i]], writes=[b_st5[i]])
                    S.dve(lambda e, i=i: e.reciprocal(out=st5s[:, i, 3:4], in_=st5s[:, i, 2:3]), reads=[b_st5[i]], writes=[b_st5[i]])
                    S.dve(lambda e, i=i, u=u: e.tensor_scalar(out=h2t[u][:], in0=x1t[u][:], scalar1=st5s[:, i, 3:4], scalar2=None, op0=ALU.mult),
                          reads=[b_x1t[u], b_st5[i]], writes=[b_h2t[u]])
                    S.pool(lambda e, u=u: e.tensor_tensor(out=h2t[u][:], in0=h2t[u][:], in1=nw2[:], op=ALU.mult), reads=[b_h2t[u]] + b_small, writes=[b_h2t[u]])
                    S.dma("sp", lambda e, u=u, r0=r0: e.dma_start(out=H2[r0:r0 + 128, :], in_=h2t[u][:]), reads=[b_h2t[u]], writes=[S.dout("h2")], waw=False)

                    def tph(e, u=u):
                        r = None
                        for kc in range(8):
                            r = e.transpose(out=ph[:, kc, :], in_=h2t[u][:, kc * 128:(kc + 1) * 128], identity=ident_f[:])
                        return r
                    S.pe(tph, reads=[b_h2t[u]], writes=[b_ph])
                    def h2cp(e):
                        e.copy(out=h2T[:, 0:4, :], in_=ph[:, 0:4, :])
                        return e.copy(out=h2T[:, 4:8, :], in_=ph[:, 4:8, :])
                    S.act(h2cp, reads=[b_ph], writes=[b_h2T])
                    k = ppi[0] % 3
                    ppi[0] += 1

                    def rmm(e, k=k):
                        r = None
                        for kc in range(8):
                            r = e.matmul(pp[k][:, 0:72], lhsT=h2T[:, kc, :], rhs=Wr[:, kc, :], start=(kc == 0), stop=(kc == 7))
                        return r
                    S.pe(rmm, reads=[b_h2T] + b_small, writes=[b_pp[k]])
                    S.dve(lambda e, k=k, i=i: e.tensor_tensor(out=LG[:, i, :], in0=pp[k][:, 0:72], in1=brb[:], op=ALU.add),
                          reads=[b_pp[k]] + b_small, writes=[b_LG[i]])
                S.flush()
            if stop_after <= 5:
                return nc, S

            with contextlib.ExitStack() as st6:
                def sb6(name, shape, dt):
                    return st6.enter_context(nc.sbuf_tensor(name, list(shape), dt))

                def ps6(name, shape, dt):
                    return st6.enter_context(nc.psum_tensor(name, list(shape), dt))
                R = S.buf("R")
                gmax = sb6("gmax", [128, NT], F32)
                goh = sb6("goh", [128, NT, 8], F32)
                gex = sb6("gex", [128, NT, 8], F32)
                gsum = sb6("gsum", [128, NT], F32)
                ggate = sb6("ggate", [128, NT], F32)
                tmp64 = sb6("tmp64", [128, NT, 8, 8], F32)
                esel = sb6("esel", [128, NT, 8], F32)
                e2 = sb6("e2", [128, NT, 8], F32)
                m1_ = sb6("m1_", [128, NT], F32)
                m2_ = sb6("m2_", [128, NT], F32)
                oh1 = sb6("oh1", [128, NT, 8], F32)
                oh2 = sb6("oh2", [128, NT, 8], F32)
                dd_ = sb6("dd_", [128, NT], F32)
                A0 = sb6("A0", [128, NT, 64], F32)
                A1 = sb6("A1", [128, NT, 64], F32)
                Ab = sb6("Ab", [128, NT, 64], BF16)
                pref = sb6("pref", [128, NT, 64], F32)
                cnt = sb6("cnt", [128, 64], F32)
                padded = sb6("padded", [128, 64], F32)
                cntI = sb6("cntI", [128, 64], I32)
                pend = sb6("pend", [128, 64], F32)
                pstart = sb6("pstart", [128, 64], F32)
                dest = sb6("dest", [128, 2, NT], F32)
                bval = sb6("bval", [128, 128], F32)
                bvalI = sb6("bvalI", [128, 128], I32)
                pidxI = sb6("pidxI", [128, 1], I32)
                pidx = sb6("pidx", [128, 1], F32)
                cmp = sb6("cmp", [128, 128, 64], F32)
                blkE = sb6("blkE", [128, 128], F32)
                IDXf = sb6("IDXf", [128, 128], F32)
                ppre = [ps6(f"ppre{i}", [128, 512], F32) for i in range(2)]
                b_ppre = S.bufs(2, "ppre")
                pcnt = ps6("pcnt", [128, 512], F32)
                b_pcnt = S.buf("pcnt")
                LGg = LG[:, :, 0:8]
                LGe = LG[:, :, 8:72].rearrange("p t (g e) -> p t g e", e=8)

                S.seq("dve", [
                    lambda e: e.tensor_reduce(out=gmax[:], in_=LGg, axis=AX.X, op=ALU.max),
                    lambda e: e.tensor_tensor(out=goh[:], in0=LGg, in1=bc3(gmax[:, :], 8), op=ALU.is_equal),
                    lambda e: e.tensor_tensor(out=gex[:], in0=LGg, in1=bc3(gmax[:, :], 8), op=ALU.subtract),
                ], reads=b_LG, writes=[R])
                S.act(lambda e: e.activation(out=gex[:], in_=gex[:], func=AF.Exp), reads=[R], writes=[R])

                S.seq("dve", [
                    lambda e: e.tensor_reduce(out=gsum[:], in_=gex[:], axis=AX.X, op=ALU.add),
                    lambda e: e.reciprocal(out=ggate[:], in_=gsum[:]),
                    lambda e: e.tensor_tensor(out=tmp64[:], in0=LGe, in1=goh[:].unsqueeze(3).to_broadcast([128, NT, 8, 8]), op=ALU.mult),
                    lambda e: e.tensor_reduce(out=esel[:], in_=tmp64[:].rearrange("p t g e -> p t e g"), axis=AX.X, op=ALU.add),
                    lambda e: e.tensor_reduce(out=m1_[:], in_=esel[:], axis=AX.X, op=ALU.max),
                    lambda e: e.tensor_tensor(out=oh1[:], in0=esel[:], in1=bc3(m1_[:, :], 8), op=ALU.is_equal),
                    lambda e: e.scalar_tensor_tensor(out=e2[:], in0=oh1[:], scalar=-1e30, in1=esel[:], op0=ALU.mult, op1=ALU.add),
                    lambda e: e.tensor_reduce(out=m2_[:], in_=e2[:], axis=AX.X, op=ALU.max),
                    lambda e: e.tensor_tensor(out=oh2[:], in0=e2[:], in1=bc3(m2_[:, :], 8), op=ALU.is_equal),
                    lambda e: e.tensor_tensor(out=dd_[:], in0=m2_[:], in1=m1_[:], op=ALU.subtract),
                ], reads=[R] + b_LG, writes=[R])
                S.act(lambda e: e.activation(out=dd_[:], in_=dd_[:], func=AF.Exp), reads=[R], writes=[R])

                S.seq("dve", [
                    lambda e: e.tensor_scalar(out=dd_[:], in0=dd_[:], scalar1=1.0, scalar2=None, op0=ALU.add),
                    lambda e: e.reciprocal(out=W1[:], in_=dd_[:]),
                    lambda e: e.tensor_scalar(out=W2[:], in0=W1[:], scalar1=-1.0, scalar2=1.0, op0=ALU.mult, op1=ALU.add),
                    lambda e: e.tensor_tensor(out=W1[:], in0=W1[:], in1=ggate[:], op=ALU.mult),
                    lambda e: e.tensor_tensor(out=W2[:], in0=W2[:], in1=ggate[:], op=ALU.mult),
                    lambda e: e.tensor_tensor(out=A0[:].rearrange("p t (g e) -> p t g e", e=8), in0=goh[:].unsqueeze(3).to_broadcast([128, NT, 8, 8]), in1=oh1[:].unsqueeze(2).to_broadcast([128, NT, 8, 8]), op=ALU.mult),
                    lambda e: e.tensor_tensor(out=A1[:].rearrange("p t (g e) -> p t g e", e=8), in0=goh[:].unsqueeze(3).to_broadcast([128, NT, 8, 8]), in1=oh2[:].unsqueeze(2).to_broadcast([128, NT, 8, 8]), op=ALU.mult),
                    lambda e: e.tensor_tensor(out=Ab[:], in0=A0[:], in1=A1[:], op=ALU.add),
                ], reads=[R], writes=[R])

                def cntf(e):
                    r = None
                    for T in range(NT):
                        r = e.matmul(pcnt[:, 0:64], lhsT=ones_b[:], rhs=Ab[:, T, :], start=(T == 0), stop=(T == NT - 1))
                    return r
                S.pe(cntf, reads=[R], writes=[b_pcnt])
                prefbufs = S.bufs(NT, "pref")
                for T in range(NT):
                    k = T % 2

                    def pref_f(e, T=T, k=k):
                        r = e.matmul(ppre[k][:, 0:64], lhsT=striu_b[:], rhs=Ab[:, T, :], start=True, stop=(T == 0))
                        for T2 in range(T):
                            r = e.matmul(ppre[k][:, 0:64], lhsT=ones_b[:], rhs=Ab[:, T2, :], start=False, stop=(T2 == T - 1))
                        return r
                    S.pe(pref_f, reads=[R], writes=[b_ppre[k]])
                    S.act(lambda e, T=T, k=k: e.copy(out=pref[:, T, :], in_=ppre[k][:, 0:64]), reads=[b_ppre[k]], writes=[prefbufs[T]])

                S.seq("dve", [
                    lambda e: e.tensor_copy(out=cnt[:], in_=pcnt[:, 0:64]),
                    lambda e: e.tensor_scalar(out=pend[:], in0=cnt[:], scalar1=127.0, scalar2=1.0 / 128, op0=ALU.add, op1=ALU.mult),
                    lambda e: e.tensor_copy(out=cntI[:], in_=pend[:]),
                    lambda e: e.tensor_copy(out=padded[:], in_=cntI[:]),
                    lambda e: e.tensor_tensor(out=pstart[:], in0=padded[:], in1=pend[:], op=ALU.is_gt),
                    lambda e: e.tensor_tensor(out=padded[:], in0=padded[:], in1=pstart[:], op=ALU.subtract),
                    lambda e: e.tensor_scalar(out=padded[:], in0=padded[:], scalar1=128.0, scalar2=None, op0=ALU.mult),
                    lambda e: e.tensor_tensor_scan(out=pend[:], data0=ones_f[:, 0:64], data1=padded[:], initial=0.0, op0=ALU.mult, op1=ALU.add),
                    lambda e: e.tensor_tensor(out=pstart[:], in0=pend[:], in1=padded[:], op=ALU.subtract),
                    lambda e: e.tensor_tensor(out=pref[:], in0=pref[:], in1=bcmid(pstart[:, :], NT), op=ALU.add),
                    lambda e: e.tensor_tensor(out=A0[:], in0=A0[:], in1=pref[:], op=ALU.mult),
                    lambda e: e.tensor_reduce(out=dest[:, 0, :], in_=A0[:], axis=AX.X, op=ALU.add),
                    lambda e: e.tensor_tensor(out=A1[:], in0=A1[:], in1=pref[:], op=ALU.mult),
                    lambda e: e.tensor_reduce(out=dest[:, 1, :], in_=A1[:], axis=AX.X, op=ALU.add),
                    lambda e: e.tensor_copy(out=destI[:], in_=dest[:]),
                ], reads=[R, b_pcnt] + prefbufs, writes=[R])

                def r5(e):
                    e.iota(bvalI[:], pattern=[[128, 128]], base=0, channel_multiplier=0)
                    return e.iota(pidxI[:], pattern=[[0, 1]], base=0, channel_multiplier=1)
                b_iota = S.buf("iota")
                S.pool(r5, writes=[b_iota])

                S.seq("dve", [
                    lambda e: e.tensor_copy(out=bval[:], in_=bvalI[:]),
                    lambda e: e.tensor_copy(out=pidx[:], in_=pidxI[:]),
                    lambda e: e.tensor_tensor(out=cmp[:], in0=bcmid(pend[:, :], 128), in1=bc3(bval[:, :], 64), op=ALU.is_le),
                    lambda e: e.tensor_reduce(out=blkE[:], in_=cmp[:], axis=AX.X, op=ALU.add),
                    lambda e: e.tensor_scalar(out=blkE[:], in0=blkE[:], scalar1=63.0, scalar2=None, op0=ALU.min),
                    lambda e: e.tensor_scalar(out=IDXf[:], in0=blkE[:], scalar1=128.0, scalar2=None, op0=ALU.mult),
                    lambda e: e.tensor_scalar(out=IDXf[:], in0=IDXf[:], scalar1=pidx[:, 0:1], scalar2=None, op0=ALU.add),
                    lambda e: e.tensor_copy(out=IDX[:], in_=IDXf[:]),
                ], reads=[R, b_iota], writes=[R])
                if debug:
                    def rdbg(e):
                        e.tensor_copy(out=tmp64[:, :, 0, 0:1], in_=dest[:, 0, :].unsqueeze(2))
                        e.tensor_copy(out=tmp64[:, :, 0, 1:2], in_=dest[:, 1, :].unsqueeze(2))
                        e.tensor_copy(out=tmp64[:, :, 0, 2:3], in_=W1[:].unsqueeze(2))
                        e.tensor_copy(out=tmp64[:, :, 0, 3:4], in_=W2[:].unsqueeze(2))
                        e.tensor_copy(out=tmp64[:, :, 0, 4:5], in_=blkE[:, 0:NT].unsqueeze(2))
                        e.tensor_copy(out=tmp64[:, :, 0, 5:6], in_=blkE[:, 32:64].unsqueeze(2))
                        e.tensor_copy(out=tmp64[:, :, 0, 6:7], in_=cnt[:, 0:NT].unsqueeze(2))
                        return e.tensor_copy(out=tmp64[:, :, 0, 7:8], in_=cnt[:, 32:64].unsqueeze(2))
                    S.dve(rdbg, reads=[R], writes=[R])
                    S.dma("sp", lambda e: e.dma_start(out=RT, in_=tmp64[:, :, 0, :]), reads=[R], writes=[S.dout("rt")])

                h2s = [sb6(f"h2s{i}", [128, 1024], F32) for i in range(2)]
                b_h2s = S.bufs(2, "h2s")
                b_xbuf = S.dout("xbuf")
                for T in range(NT):
                    u = T % 2
                    S.dma("sp", lambda e, u=u, T=T: e.dma_start(out=h2s[u][:], in_=H2[T * 128:(T + 1) * 128, :]), writes=[b_h2s[u]])
                    for k in range(2):
                        S.dma("pool", lambda e, u=u, T=T, k=k: e.indirect_dma_start(
                            out=XBUF, out_offset=bass.IndirectOffsetOnAxis(ap=destI[:, k, T:T + 1], axis=0), in_=h2s[u][:], in_offset=None),
                            reads=[b_h2s[u], R], writes=[b_xbuf], waw=False)
                S.flush()

            with contextlib.ExitStack() as st7:
                def sb7(name, shape, dt):
                    return st7.enter_context(nc.sbuf_tensor(name, list(shape), dt))

                def ps7(name, shape, dt):
                    return st7.enter_context(nc.psum_tensor(name, list(shape), dt))
                R2 = S.buf("R2")
                wg = [sb7(f"wg{i}", [128, 4096], F32) for i in range(2)]
                wu = [sb7(f"wu{i}", [128, 4096], F32) for i in range(2)]
                wdn = [sb7(f"wd{i}", [128, 4096], F32) for i in range(2)]
                b_wg = S.bufs(2, "wg")
                b_wu = S.bufs(2, "wu")
                b_wd = S.bufs(2, "wd")
                wgb = [sb7(f"wgb{i}", [128, 4096], BF16) for i in range(2)]
                wub = [sb7(f"wub{i}", [128, 4096], BF16) for i in range(2)]
                wdb = [sb7(f"wdb{i}", [128, 4096], BF16) for i in range(2)]
                b_wgb = S.bufs(2, "wgb")
                b_wub = S.bufs(2, "wub")
                b_wdbA = S.bufs(2, "wdbA")
                b_wdbB = S.bufs(2, "wdbB")
                xbt = [sb7(f"xbt{i}", [128, 1024], F32) for i in range(2)]
                b_xbt = S.bufs(2, "xbt")
                xbT = sb7("xbT", [128, 8, 128], BF16)
                b_xbT = S.buf("xbT")
                sg = sb7("sg", [128, 512], F32)
                hid = sb7("hid", [128, 512], F32)
                hdT = sb7("hdT", [128, 4, 128], BF16)
                b_sg = S.buf("sg")
                b_hid = S.buf("hid")
                b_hdT = S.buf("hdT")
                yb = [sb7(f"yb{i}", [128, 1024], F32) for i in range(2)]
                b_yb = S.bufs(2, "yb")
                pxt = ps7("pxt", [128, 8, 128], F32)
                b_pxt = S.buf("pxt")
                pg = ps7("pg", [128, 512], F32)
                pu = ps7("pu", [128, 512], F32)
                b_pg = S.buf("pg")
                b_pu = S.buf("pu")
                pht = ps7("pht", [128, 4, 128], F32)
                b_pht = S.buf("pht")
                pyd = [ps7(f"pyd{i}", [128, 512], F32) for i in range(2)]
                b_pyd = S.bufs(2, "pyd")
                b_ybuf = S.dout("ybuf")
                for b in range(128):
                    u = b % 2
                    for (wsrc, wt, bw) in ((w_g, wg, b_wg), (w_u, wu, b_wu), (w_d, wdn, b_wd)):
                        S.dma("pool", lambda e, wsrc=wsrc, wt=wt, u=u, b=b: e.indirect_dma_start(
                            out=wt[u][:], out_offset=None, in_=wsrc, in_offset=bass.IndirectOffsetOnAxis(ap=IDX[:, b:b + 1], axis=0)),
                            reads=[R2], writes=[bw[u]])
                    S.dma("sp", lambda e, u=u, b=b: e.dma_start(out=xbt[u][:], in_=XBUF[b * 128:(b + 1) * 128, :]), reads=[R2], writes=[b_xbt[u]])
                    S.act(lambda e, u=u: e.copy(out=wgb[u][:], in_=wg[u][:]), reads=[b_wg[u]], writes=[b_wgb[u]])
                    S.dve(lambda e, u=u: e.tensor_copy(out=wub[u][:], in_=wu[u][:]), reads=[b_wu[u]], writes=[b_wub[u]])
                    S.dve(lambda e, u=u: e.tensor_copy(out=wdb[u][:, 0:2048], in_=wdn[u][:, 0:2048]), reads=[b_wd[u]], writes=[b_wdbA[u]])
                    S.act(lambda e, u=u: e.copy(out=wdb[u][:, 2048:4096], in_=wdn[u][:, 2048:4096]), reads=[b_wd[u]], writes=[b_wdbB[u]])

                    def tpx(e, u=u):
                        r = None
                        for kc in range(8):
                            r = e.transpose(out=pxt[:, kc, :], in_=xbt[u][:, kc * 128:(kc + 1) * 128], identity=ident_f[:])
                        return r
                    S.pe(tpx, reads=[b_xbt[u]], writes=[b_pxt])
                    def xbcp(e):
                        e.copy(out=xbT[:, 0:4, :], in_=pxt[:, 0:4, :])
                        return e.copy(out=xbT[:, 4:8, :], in_=pxt[:, 4:8, :])
                    S.act(xbcp, reads=[b_pxt], writes=[b_xbT])

                    def gu(e, u=u):
                        r = None
                        for kc in range(8):
                            r = e.matmul(pg[:], lhsT=xbT[:, kc, :], rhs=wgb[u][:, kc * 512:(kc + 1) * 512], start=(kc == 0), stop=(kc == 7))
                        for kc in range(8):
                            r = e.matmul(pu[:], lhsT=xbT[:, kc, :], rhs=wub[u][:, kc * 512:(kc + 1) * 512], start=(kc == 0), stop=(kc == 7))
                        return r
                    S.pe(gu, reads=[b_xbT, b_wgb[u], b_wub[u]], writes=[b_pg, b_pu])
                    S.act(lambda e: e.activation(out=sg[:], in_=pg[:], func=AF.Silu), reads=[b_pg], writes=[b_sg])
                    S.dve(lambda e: e.tensor_tensor(out=hid[:], in0=pu[:], in1=sg[:], op=ALU.mult), reads=[b_pu, b_sg], writes=[b_hid])

                    def tphd(e):
                        r = None
                        for kc in range(4):
                            r = e.transpose(out=pht[:, kc, :], in_=hid[:, kc * 128:(kc + 1) * 128], identity=ident_f[:])
                        return r
                    S.pe(tphd, reads=[b_hid], writes=[b_pht])
                    S.act(lambda e: e.copy(out=hdT[:], in_=pht[:]), reads=[b_pht], writes=[b_hdT])
                    for half in range(2):
                        def dn(e, u=u, half=half):
                            r = None
                            for kc in range(4):
                                r = e.matmul(pyd[half][:], lhsT=hdT[:, kc, :], rhs=wdb[u][:, kc * 1024 + half * 512:kc * 1024 + half * 512 + 512],
                                             start=(kc == 0), stop=(kc == 3))
                            return r
                        S.pe(dn, reads=[b_hdT, b_wdbA[u], b_wdbB[u]], writes=[b_pyd[half]])
                        if half == 0:
                            S.dve(lambda e, u=u: e.tensor_copy(out=yb[u][:, 0:512], in_=pyd[0][:]), reads=[b_pyd[0]], writes=[b_yb[u]])
                        else:
                            S.act(lambda e, u=u: e.copy(out=yb[u][:, 512:1024], in_=pyd[1][:]), reads=[b_pyd[1]], writes=[b_yb[u]])
                    S.dma("sp", lambda e, u=u, b=b: e.dma_start(out=YBUF[b * 128:(b + 1) * 128, :], in_=yb[u][:]), reads=[b_yb[u]], writes=[b_ybuf], waw=False)
                S.flush()

            with contextlib.ExitStack() as st8:
                def sb8(name, shape, dt):
                    return st8.enter_context(nc.sbuf_tensor(name, list(shape), dt))
                R3 = S.buf("R3")
                fnb = sb8("fnb", [128, 1024], F32)
                b_fnb = S.buf("fnb")
                S.dma("sp", lambda e: e.dma_start(out=fnb[:], in_=fnw.partition_broadcast(128)), writes=[b_fnb])
                y0 = [sb8(f"y0_{i}", [128, 1024], F32) for i in range(2)]
                y1 = [sb8(f"y1_{i}", [128, 1024], F32) for i in range(2)]
                xx = [sb8(f"xx{i}", [128, 1024], F32) for i in range(2)]
                b_y0 = S.bufs(2, "y0")
                b_y1 = S.bufs(2, "y1")
                b_xx = S.bufs(2, "xx")
                acc = [sb8(f"acc{i}", [128, 1024], F32) for i in range(2)]
                b_acc = S.bufs(2, "acc")
                junk8 = sb8("junk8", [128, 1024], BF16)
                b_junk8 = S.buf("junk8")
                st8s = sb8("st8s", [128, NT, 4], F32)
                b_st8 = S.bufs(NT, "st8")
                ot = [sb8(f"ot{i}", [128, 1024], F32) for i in range(2)]
                b_ot = S.bufs(2, "ot")
                b_out = S.dout("out")
                for T in range(NT):
                    u = T % 2
                    S.dma("pool", lambda e, u=u, T=T: e.indirect_dma_start(out=y0[u][:], out_offset=None, in_=YBUF,
                                                                            in_offset=bass.IndirectOffsetOnAxis(ap=destI[:, 0, T:T + 1], axis=0)),
                          reads=[R3], writes=[b_y0[u]])
                    S.dma("pool", lambda e, u=u, T=T: e.indirect_dma_start(out=y1[u][:], out_offset=None, in_=YBUF,
                                                                            in_offset=bass.IndirectOffsetOnAxis(ap=destI[:, 1, T:T + 1], axis=0)),
                          reads=[R3], writes=[b_y1[u]])
                    S.dma("sp", lambda e, u=u, T=T: e.dma_start(out=xx[u][:], in_=X1[T * 128:(T + 1) * 128, :]), writes=[b_xx[u]])
                    S.dve(lambda e, u=u, T=T: e.scalar_tensor_tensor(out=acc[u][:], in0=y0[u][:], scalar=W1[:, T:T + 1], in1=xx[u][:], op0=ALU.mult, op1=ALU.add),
                          reads=[b_y0[u], b_xx[u], R3], writes=[b_acc[u]])
                    S.dve(lambda e, u=u, T=T: e.scalar_tensor_tensor(out=acc[u][:], in0=y1[u][:], scalar=W2[:, T:T + 1], in1=acc[u][:], op0=ALU.mult, op1=ALU.add),
                          reads=[b_y1[u], b_acc[u], R3], writes=[b_acc[u]])
                    S.act(lambda e, u=u, T=T: e.activation(out=junk8[:], in_=acc[u][:], func=AF.Square, accum_out=st8s[:, T, 0:1]),
                          reads=[b_acc[u]], writes=[b_junk8, b_st8[T]])
                    S.dve(lambda e, T=T: e.tensor_scalar(out=st8s[:, T, 1:2], in0=st8s[:, T, 0:1], scalar1=1.0 / D, scalar2=1e-6, op0=ALU.mult, op1=ALU.add),
                          reads=[b_st8[T]], writes=[b_st8[T]])
                    S.act(lambda e, T=T: e.activation(out=st8s[:, T, 2:3], in_=st8s[:, T, 1:2], func=AF.Sqrt), reads=[b_st8[T]], writes=[b_st8[T]])
                    S.dve(lambda e, T=T: e.reciprocal(out=st8s[:, T, 3:4], in_=st8s[:, T, 2:3]), reads=[b_st8[T]], writes=[b_st8[T]])
                    S.dve(lambda e, u=u, T=T: e.tensor_scalar(out=acc[u][:], in0=acc[u][:], scalar1=st8s[:, T, 3:4], scalar2=None, op0=ALU.mult),
                          reads=[b_acc[u], b_st8[T]], writes=[b_acc[u]])
                    S.pool(lambda e, u=u: e.tensor_tensor(out=ot[u][:], in0=acc[u][:], in1=fnb[:], op=ALU.mult), reads=[b_acc[u], b_fnb], writes=[b_ot[u]])
                    S.dma("sp", lambda e, u=u, T=T: e.dma_start(out=out[T * 128:(T + 1) * 128, :], in_=ot[u][:]), reads=[b_ot[u]], writes=[b_out], waw=False)
                S.flush(final=True)
    return nc, S


def prep_inputs(inp):
    f32 = np.float32
    w_in = np.asarray(inp["w_in"], f32)[0]
    offs = np.cumsum([0, 1024, 1024, 1024, 2048, 4096, 32, 1024, 1024])
    wq, wk, wv, wz, wx, wdt, wga, wgs = [w_in[:, offs[i]:offs[i + 1]] for i in range(8)]
    perm = np.arange(1024).reshape(8, 2, 64)
    perm = np.concatenate([perm[:, :, 32:], perm[:, :, :32]], axis=2).reshape(-1)

    def inter(w):
        ws = w[:, perm]
        return np.concatenate([np.concatenate([w[:, h * 128:(h + 1) * 128], ws[:, h * 128:(h + 1) * 128]], axis=1) for h in range(8)], axis=1)
    pad = np.zeros((1024, WCOLS - (2048 * 2 + 4096 + 1024 + 2048 + 2048 + 32)), f32)
    w2 = np.ascontiguousarray(np.concatenate([inter(wq), inter(wk), wx, wv, wz, wga, wgs, wdt, pad], axis=1))
    assert w2.shape == (1024, WCOLS), w2.shape
    com = {}
    com["w_in"] = w2
    com["normw1"] = np.ascontiguousarray(np.asarray(inp["norm_mix_w"], f32)[0].reshape(8, 128).T)
    com["convw"] = np.ascontiguousarray(np.asarray(inp["conv_w"], f32)[0].reshape(4, 32, 128).transpose(2, 1, 0))
    com["convb"] = np.ascontiguousarray(np.asarray(inp["conv_b"], f32)[0].reshape(32, 128).T)
    com["dt_bias"] = np.ascontiguousarray(np.asarray(inp["dt_bias"], f32)[0])
    com["a_log"] = np.ascontiguousarray(np.asarray(inp["a_log"], f32)[0])
    com["d_skip"] = np.ascontiguousarray(np.asarray(inp["d_skip"], f32)[0])
    com["ssd_nw"] = np.ascontiguousarray(np.asarray(inp["ssd_norm_w"], f32)[0])
    com["lam4"] = np.ascontiguousarray(np.concatenate([np.asarray(inp[k], f32)[0] for k in ("lambda_q1", "lambda_k1", "lambda_q2", "lambda_k2")]))
    com["subln"] = np.ascontiguousarray(np.asarray(inp["subln_w"], f32)[0])
    com["w_pa"] = np.ascontiguousarray(np.asarray(inp["w_branch_attn"], f32)[0])
    com["w_ps"] = np.ascontiguousarray(np.asarray(inp["w_branch_ssd"], f32)[0])
    com["w_o"] = np.ascontiguousarray(np.asarray(inp["w_out"], f32)[0])
    com["normw2"] = np.ascontiguousarray(np.asarray(inp["norm_ffn_w"], f32)[0])
    com["w_r"] = np.ascontiguousarray(np.concatenate([np.asarray(inp["w_group_router"], f32)[0], np.asarray(inp["w_expert_router"], f32)[0]], axis=1))
    com["b_r"] = np.ascontiguousarray(np.concatenate([np.asarray(inp["b_group_router"], f32)[0], np.asarray(inp["b_expert_router"], f32)[0]]))
    com["w_g"] = np.ascontiguousarray(np.asarray(inp["w_expert_gate"], f32)[0].reshape(64, 8, 128, 512).transpose(0, 2, 1, 3)).reshape(8192, 4096)
    com["w_u"] = np.ascontiguousarray(np.asarray(inp["w_expert_up"], f32)[0].reshape(64, 8, 128, 512).transpose(0, 2, 1, 3)).reshape(8192, 4096)
    com["w_d"] = np.ascontiguousarray(np.asarray(inp["w_expert_down"], f32)[0].reshape(64, 4, 128, 1024).transpose(0, 2, 1, 3)).reshape(8192, 4096)
    com["fnw"] = np.ascontiguousarray(np.asarray(inp["final_norm_w"], f32))
    p = np.arange(128)
    invf = (1.0 / (10000.0 ** (np.arange(0, 64, 2, dtype=np.float32) / 64.0))).astype(f32)
    sgn = np.where((p % 64) < 32, -1.0, 1.0).astype(f32)
    com["rconst"] = np.ascontiguousarray(np.stack([invf[p % 32], sgn], axis=1).astype(f32))
    xs = np.asarray(inp["x"], f32)
    ps_ = np.asarray(inp["positions"], np.int32)
    maps = []
    for c in range(NCORES):
        m = dict(com)
        m["x"] = np.ascontiguousarray(xs[2 * c:2 * c + 2].reshape(TOK, D))
        m["pos"] = np.ascontiguousarray(ps_[2 * c:2 * c + 2].reshape(TOK))
        maps.append(m)
    return maps


_CACHE = {}


def kernel(**inputs):
    maps = prep_inputs(inputs)
    if "nc" not in _CACHE:
        _CACHE["nc"] = build_program()[0]
    nc = _CACHE["nc"]
    res = run_bass_kernel_spmd(nc, maps, core_ids=list(range(NCORES)))
    outs = [np.asarray(r["out"], np.float32).reshape(2, 2048, D) for r in res.results]
    return np.concatenate(outs, axis=0)
```

```python
import contextlib
import math
import numpy as np
import concourse.bass as bass
import concourse.mybir as mybir
from concourse.bass_utils import run_bass_kernel_spmd

F32 = mybir.dt.float32
BF16 = mybir.dt.bfloat16
I32 = mybir.dt.int32
ALU = mybir.AluOpType
AF = mybir.ActivationFunctionType
AX = mybir.AxisListType

NCORES = 8
TOK = 4096
NT = 32
D = 1024
WCOLS = 13344
PI = math.pi


class Buf:
    __slots__ = ("name", "writers", "readers", "lane", "swlane", "is_dram")

    def __init__(self, name):
        self.name = name
        self.is_dram = False
        self.swlane = None
        self.writers = []
        self.readers = []
        self.lane = None


class Op:
    __slots__ = ("eng", "fn", "deps", "signaled", "is_dma", "token", "lane")

    def __init__(self, eng, fn, is_dma):
        self.eng = eng
        self.fn = fn
        self.deps = set()
        self.signaled = False
        self.is_dma = is_dma
        self.token = None
        self.lane = None


class Sched:
    ENGS = ("pe", "act", "dve", "pool", "sp")

    def __init__(self, nc, st, nlanes=56):
        self.nc = nc
        self.ops = []
        self.allbufs = []
        self.esem = {e: st.enter_context(nc.semaphore(f"s_{e}")) for e in ("pe", "act", "dve", "pool")}
        self.ecount = {e: 0 for e in self.esem}
        self.bar = st.enter_context(nc.semaphore("s_bar"))
        self.barcount = 0
        self.lanes = [[st.enter_context(nc.semaphore(f"l{i}")), 0] for i in range(nlanes)]
        self.swlanes = [[st.enter_context(nc.semaphore(f"w{i}")), 0] for i in range(8)]
        self.waited = {e: {} for e in self.ENGS}
        self.eng = {"pe": nc.tensor, "act": nc.scalar, "dve": nc.vector, "pool": nc.gpsimd, "sp": nc.sync}
        self.nins = 0
        self.douts = {}

    def buf(self, name="b"):
        b = Buf(name)
        self.allbufs.append(b)
        return b

    def dout(self, name):
        if name not in self.douts:
            self.douts[name] = self.buf(name)
            self.douts[name].is_dram = True
        return self.douts[name]

    def bufs(self, n, name="b"):
        return [self.buf(f"{name}{i}") for i in range(n)]

    def op(self, eng, fn, reads=(), writes=(), dma=False, waw=True):
        o = Op(eng, fn, dma)
        oid = len(self.ops)
        for b in reads:
            o.deps.update(b.writers)
        for b in writes:
            o.deps.update(b.readers)
            if waw:
                o.deps.update(b.writers)
        for b in reads:
            b.readers.append(oid)
        for b in writes:
            if waw:
                b.writers = [oid]
                b.readers = []
            else:
                b.writers.append(oid)
        if dma:
            o.lane = reads[0] if writes[0].is_dram else writes[0]
            assert not o.lane.is_dram
        o.deps.discard(oid)
        self.ops.append(o)
        return oid

    def seq(self, eng, fns, reads=(), writes=()):
        for fn in fns:
            self.op(eng, fn, reads, writes)

    def pe(self, fn, reads=(), writes=()):
        return self.op("pe", fn, reads, writes)

    def act(self, fn, reads=(), writes=()):
        return self.op("act", fn, reads, writes)

    def dve(self, fn, reads=(), writes=()):
        return self.op("dve", fn, reads, writes)

    def pool(self, fn, reads=(), writes=()):
        return self.op("pool", fn, reads, writes)

    def dma(self, eng, fn, reads=(), writes=(), waw=True):
        return self.op(eng, fn, reads, writes, dma=True, waw=waw)

    def flush(self, final=False):
        ops = self.ops
        for o in ops:
            for d in o.deps:
                ops[d].signaled = True
        last = {}
        for oid, o in enumerate(ops):
            if not o.is_dma:
                last[o.eng] = oid
        for oid in last.values():
            ops[oid].signaled = True
        free_lanes = list(range(len(self.lanes)))
        free_sw = list(range(len(self.swlanes)))
        used_lanes = []
        for oid, o in enumerate(ops):
            eo = self.eng[o.eng]
            need = {}
            for d in o.deps:
                dop = ops[d]
                if (not dop.is_dma) and dop.eng == "pe" and o.eng == "pe" and not o.is_dma:
                    continue
                sem, val = dop.token
                k = id(sem)
                if k not in need or need[k][1] < val:
                    need[k] = (sem, val)
            for k, (sem, val) in need.items():
                if self.waited[o.eng].get(k, 0) >= val:
                    continue
                eo.wait_ge(sem, val)
                self.waited[o.eng][k] = val
            ins = o.fn(eo)
            self.nins += 1
            if o.is_dma:
                lb = o.lane
                if o.eng == "pool":
                    if lb.swlane is None:
                        lb.swlane = free_sw.pop(0)
                        used_lanes.append(self.swlanes[lb.swlane])
                    ln = self.swlanes[lb.swlane]
                else:
                    if lb.lane is None:
                        lb.lane = free_lanes.pop(0)
                        used_lanes.append(self.lanes[lb.lane])
                    ln = self.lanes[lb.lane]
                ln[1] += 16
                ins.then_inc(ln[0], 16)
                o.token = (ln[0], ln[1])
            elif o.signaled:
                self.ecount[o.eng] += 1
                ins.then_inc(self.esem[o.eng], 1)
                o.token = (self.esem[o.eng], self.ecount[o.eng])
        sp = self.nc.sync
        for e in ("pe", "act", "dve", "pool"):
            if self.ecount[e] > 0:
                sp.wait_ge(self.esem[e], self.ecount[e])
        for ln in used_lanes:
            sp.wait_ge(ln[0], ln[1])
        if not final:
            self.barcount += 1
            sp.sem_inc(self.bar, 1)
            for e in ("pe", "act", "dve", "pool"):
                self.eng[e].wait_ge(self.bar, self.barcount)
        self.ops = []
        for b in self.allbufs:
            b.writers = []
            b.readers = []
            b.lane = None
            b.swlane = None


def bc3(ap2, n):
    p, a = ap2.shape
    return ap2.unsqueeze(2).to_broadcast([p, a, n])


def bcmid(ap2, n):
    p, a = ap2.shape
    return ap2.unsqueeze(1).to_broadcast([p, n, a])


def build_program(debug=False, stop_after=99):
    nc = bass.Bass("TRN2", target_bir_lowering=False)

    def din(name, shape, dt=F32):
        return nc.dram_tensor(name, list(shape), dt, kind="ExternalInput").ap()

    def dscr(name, shape, dt=F32):
        kind = "ExternalOutput" if debug else "Internal"
        return nc.dram_tensor(name, list(shape), dt, kind=kind).ap()

    x = din("x", [TOK, D])
    pos = din("pos", [TOK], I32)
    w_in = din("w_in", [D, WCOLS])
    normw1 = din("normw1", [128, 8])
    convw = din("convw", [128, 32, 4])
    convb = din("convb", [128, 32])
    dt_bias = din("dt_bias", [32])
    a_log = din("a_log", [32])
    d_skip = din("d_skip", [32])
    ssd_nw = din("ssd_nw", [2048])
    lam4 = din("lam4", [256])
    subln = din("subln", [128])
    w_pa = din("w_pa", [1024, 1024])
    w_ps = din("w_ps", [2048, 1024])
    w_o = din("w_o", [1024, 1024])
    normw2 = din("normw2", [1024])
    w_r = din("w_r", [1024, 72])
    b_r = din("b_r", [72])
    if stop_after > 5:
        w_g = din("w_g", [8192, 4096])
        w_u = din("w_u", [8192, 4096])
        w_d = din("w_d", [8192, 4096])
    fnw = din("fnw", [1024])
    rconst = din("rconst", [128, 2])
    out = nc.dram_tensor("out", [TOK, D], F32, kind="ExternalOutput").ap()

    QT = dscr("QT", [8, 128, TOK], BF16)
    KT = dscr("KT", [8, 128, TOK], BF16)
    XBCT = dscr("XBCT", [32, 128, TOK], BF16)
    VT = dscr("VT", [TOK, 1024], BF16)
    ZS = dscr("ZS", [TOK, 2048], BF16)
    GA = dscr("GA", [TOK, 1024], BF16)
    GS = dscr("GS", [TOK, 1024], BF16)
    DTR = dscr("DTR", [TOK, 32], F32)
    ATT = dscr("ATT", [TOK, 1024], BF16)
    SSD = dscr("SSD", [TOK, 2048], BF16)
    X1 = dscr("X1", [TOK, D], F32)
    H2 = dscr("H2", [TOK, D], F32)
    RT = dscr("RT", [128, 32, 8], F32)
    XBUF = nc.dram_tensor("XBUF", [16384, D], F32, kind="Internal").ap()
    YBUF = nc.dram_tensor("YBUF", [16384, D], F32, kind="Internal").ap()

    with contextlib.ExitStack() as gst:
        S = Sched(nc, gst)

        def sbg(name, shape, dt):
            return gst.enter_context(nc.sbuf_tensor(name, list(shape), dt))

        ident_f = sbg("ident_f", [128, 128], F32)
        ident_b = sbg("ident_b", [128, 128], BF16)
        ones_f = sbg("ones_f", [128, 128], F32)
        ones_b = sbg("ones_b", [128, 128], BF16)
        tri_f = sbg("tri_f", [128, 128], F32)
        striu_b = sbg("striu_b", [128, 128], BF16)
        negmask = sbg("negmask", [128, 128], F32)
        zeros_f = sbg("zeros_f", [128, 128], F32)
        hT_all = None

        def consts():
            bC = S.buf("consts")

            S.seq("pool", [
                lambda e: e.memset(ones_f[:], 1.0),
                lambda e: e.memset(zeros_f[:], 0.0),
                lambda e: e.affine_select(out=ident_f[:], in_=ones_f[:], pattern=[[-1, 128]], compare_op=ALU.is_equal,
                                          fill=0.0, base=0, channel_multiplier=1),
                lambda e: e.affine_select(out=tri_f[:], in_=ones_f[:], pattern=[[1, 128]], compare_op=ALU.is_ge,
                                          fill=0.0, base=0, channel_multiplier=-1),
                lambda e: e.affine_select(out=negmask[:], in_=zeros_f[:], pattern=[[1, 128]], compare_op=ALU.is_ge,
                                          fill=-30000.0, base=0, channel_multiplier=-1),
                lambda e: e.affine_select(out=striu_b[:], in_=ones_f[:], pattern=[[1, 128]], compare_op=ALU.is_gt,
                                          fill=0.0, base=0, channel_multiplier=-1),
                lambda e: e.tensor_copy(out=ident_b[:], in_=ident_f[:]),
                lambda e: e.tensor_copy(out=ones_b[:], in_=ones_f[:]),
            ], writes=[bC])
            S.flush()
        consts()

        with contextlib.ExitStack() as st:
            def sb(name, shape, dt):
                return st.enter_context(nc.sbuf_tensor(name, list(shape), dt))

            def ps(name, shape, dt):
                return st.enter_context(nc.psum_tensor(name, list(shape), dt))

            hT = sb("hT_all", [128, 8, TOK], BF16)
            Ct = sb("Ct", [128, TOK], F32)
            St = sb("St", [128, TOK], F32)
            st1x = contextlib.ExitStack()
            sb_o, ps_o = sb, ps
            sb = lambda name, shape, dt: st1x.enter_context(nc.sbuf_tensor(name, list(shape), dt))
            ps = lambda name, shape, dt: st1x.enter_context(nc.psum_tensor(name, list(shape), dt))
            b_hT = S.bufs(NT, "hT")
            xt = [sb(f"xt{i}", [128, D], F32) for i in range(2)]
            b_xt = S.bufs(2, "xt")
            hb = [sb(f"hb{i}", [128, D], BF16) for i in range(2)]
            b_hb = S.bufs(2, "hb")
            junk = sb("junk", [128, D], BF16)
            b_junk = S.buf("junk")
            st1 = sb("st1", [128, NT, 4], F32)
            b_st1 = S.bufs(NT, "st1")
            ptp = [ps(f"ptp{i}", [128, 8, 128], BF16) for i in range(2)]
            b_ptp = S.bufs(2, "ptp")
            for i in range(NT):
                u = i % 2
                S.dma("sp", lambda e, i=i, u=u: e.dma_start(out=xt[u][:], in_=x[i * 128:(i + 1) * 128, :]), writes=[b_xt[u]])
                S.act(lambda e, i=i, u=u: e.activation(out=junk[:], in_=xt[u][:], func=AF.Square, accum_out=st1[:, i, 0:1]),
                      reads=[b_xt[u]], writes=[b_junk, b_st1[i]])
                S.dve(lambda e, i=i: e.tensor_scalar(out=st1[:, i, 1:2], in0=st1[:, i, 0:1], scalar1=1.0 / D, scalar2=1e-6,
                                                     op0=ALU.mult, op1=ALU.add), reads=[b_st1[i]], writes=[b_st1[i]])
                S.act(lambda e, i=i: e.activation(out=st1[:, i, 2:3], in_=st1[:, i, 1:2], func=AF.Sqrt), reads=[b_st1[i]], writes=[b_st1[i]])
                S.dve(lambda e, i=i: e.reciprocal(out=st1[:, i, 3:4], in_=st1[:, i, 2:3]), reads=[b_st1[i]], writes=[b_st1[i]])
                S.dve(lambda e, i=i, u=u: e.tensor_scalar(out=hb[u][:], in0=xt[u][:], scalar1=st1[:, i, 3:4], scalar2=None, op0=ALU.mult),
                      reads=[b_xt[u], b_st1[i]], writes=[b_hb[u]])

                def tp(e, u=u):
                    r = None
                    for kc in range(8):
                        r = e.transpose(out=ptp[u][:, kc, :], in_=hb[u][:, kc * 128:(kc + 1) * 128], identity=ident_b[:])
                    return r
                S.pe(tp, reads=[b_hb[u]], writes=[b_ptp[u]])
                S.act(lambda e, i=i, u=u: e.copy(out=hT[:, :, i * 128:(i + 1) * 128], in_=ptp[u][:]), reads=[b_ptp[u]], writes=[b_hT[i]])

            rc = sb("rc", [128, 2], F32)
            b_rc = S.buf("rc")
            posi = sb("posi", [128, TOK], I32)
            ang = sb("ang", [128, TOK], F32)
            tmpa = sb("tmpa", [128, TOK], F32)
            tmpb = sb("tmpb", [128, TOK], F32)
            negpi = sb("negpi", [128, 1], F32)
            b_rot = S.buf("rot")
            S.dma("sp", lambda e: e.dma_start(out=rc[:], in_=rconst), writes=[b_rc])
            S.dma("sp", lambda e: e.dma_start(out=posi[:], in_=pos.partition_broadcast(128)), writes=[b_rot])

            rot_fns = [lambda e: e.memset(negpi[:], -PI),
                       lambda e: e.tensor_copy(out=ang[:], in_=posi[:]),
                       lambda e: e.tensor_scalar(out=ang[:], in0=ang[:], scalar1=rc[:, 0:1], scalar2=None, op0=ALU.mult)]
            for (dst, off) in ((tmpa, 0.5), (ang, 0.75)):
                rot_fns += [
                    lambda e, dst=dst, off=off: e.tensor_scalar(out=dst[:], in0=ang[:], scalar1=1.0 / (2 * PI), scalar2=off, op0=ALU.mult, op1=ALU.add),
                    lambda e, dst=dst: e.tensor_copy(out=posi[:], in_=dst[:]),
                    lambda e: e.tensor_copy(out=tmpb[:], in_=posi[:]),
                    lambda e, dst=dst: e.tensor_tensor(out=dst[:], in0=dst[:], in1=tmpb[:], op=ALU.subtract),
                    lambda e, dst=dst: e.tensor_scalar(out=tmpb[:], in0=dst[:], scalar1=0.0, scalar2=None, op0=ALU.is_lt),
                    lambda e, dst=dst: e.tensor_tensor(out=dst[:], in0=dst[:], in1=tmpb[:], op=ALU.add)]
            S.seq("dve", rot_fns, reads=[b_rot, b_rc], writes=[b_rot])

            def rot2(e):
                e.activation(out=St[:], in_=tmpa[:], func=AF.Sin, bias=negpi[:, 0:1], scale=2 * PI)
                return e.activation(out=Ct[:], in_=ang[:], func=AF.Sin, bias=negpi[:, 0:1], scale=2 * PI)
            S.act(rot2, reads=[b_rot], writes=[b_rot])
            S.dve(lambda e: e.tensor_scalar(out=St[:], in0=St[:], scalar1=rc[:, 1:2], scalar2=None, op0=ALU.mult), reads=[b_rot, b_rc], writes=[b_rot])

            S.flush()
            st1x.close()
            if stop_after <= 1:
                return nc, S
            sb, ps = sb_o, ps_o
            b_rot = S.buf("rot2")
            nw1 = sb("nw1", [128, 8], F32)
            b_nw1 = S.buf("nw1")
            S.dma("sp", lambda e: e.dma_start(out=nw1[:], in_=normw1), writes=[b_nw1])
            wf = [sb(f"wf{i}", [128, 8, 512], F32) for i in range(2)]
            b_wf = S.bufs(2, "wf")
            wb = [sb(f"wb{i}", [128, 8, 512], BF16) for i in range(2)]
            b_wb = S.bufs(2, "wb")
            ostg = [sb(f"ostg{i}", [128, TOK], BF16) for i in range(2)]
            b_ostg = S.bufs(2, "ostg")
            t1 = [sb(f"t1_{i}", [128, 512], F32) for i in range(2)]
            b_t1 = S.bufs(2, "t1")
            t2 = [sb(f"t2_{i}", [128, 512], F32) for i in range(2)]
            b_t2 = S.bufs(2, "t2")
            tstg = [sb(f"tstg{i}", [128, 512], BF16) for i in range(3)]
            b_tstg = S.bufs(3, "tstg")
            tstf = [sb(f"tstf{i}", [128, 32], F32) for i in range(2)]
            b_tstf = S.bufs(2, "tstf")
            pf = [ps(f"pf{i}", [128, 512], F32) for i in range(4)]
            b_pf = S.bufs(4, "pf")
            w_v = w_in.rearrange("(kc p) n -> p kc n", p=128)
            all_hT = b_hT

            def load_w(g, ncols=512):
                u = g % 2
                c0 = g * 512
                S.dma("sp", lambda e: e.dma_start(out=wf[u][:, :, 0:ncols], in_=w_v[:, :, c0:c0 + ncols]), writes=[b_wf[u]])
                S.pool(lambda e: e.tensor_tensor(out=wb[u][:, :, 0:ncols], in0=wf[u][:, :, 0:ncols], in1=bc3(nw1[:, :], ncols), op=ALU.mult),
                       reads=[b_wf[u], b_nw1], writes=[b_wb[u]])
                return u

            pfi = [0]

            def mm_feat(u, c, tg):
                k = pfi[0] % 4
                pfi[0] += 1

                def f(e):
                    r = None
                    for kc in range(8):
                        r = e.matmul(pf[k][:], lhsT=wb[u][:, kc, c * 128:(c + 1) * 128], rhs=hT[:, kc, tg * 512:(tg + 1) * 512],
                                     start=(kc == 0), stop=(kc == 7))
                    return r
                S.pe(f, reads=[b_wb[u]] + all_hT[tg * 4:(tg + 1) * 4], writes=[b_pf[k]])
                return k

            evi = [0]
            for g in range(16):
                u = load_w(g)
                if g < 8:
                    for pair in range(2):
                        head = (g % 4) * 2 + pair
                        so = (g * 2 + pair) % 2
                        for tg in range(8):
                            ka = mm_feat(u, pair * 2, tg)
                            kb = mm_feat(u, pair * 2 + 1, tg)
                            tt = tg % 2
                            S.dve(lambda e, ka=ka, tg=tg, tt=tt: e.tensor_tensor(out=t1[tt][:], in0=pf[ka][:], in1=Ct[:, tg * 512:(tg + 1) * 512], op=ALU.mult),
                                  reads=[b_pf[ka], b_rot], writes=[b_t1[tt]])
                            S.dve(lambda e, kb=kb, tg=tg, tt=tt: e.tensor_tensor(out=t2[tt][:], in0=pf[kb][:], in1=St[:, tg * 512:(tg + 1) * 512], op=ALU.mult),
                                  reads=[b_pf[kb], b_rot], writes=[b_t2[tt]])
                            S.pool(lambda e, so=so, tg=tg, tt=tt: e.tensor_tensor(out=ostg[so][:, tg * 512:(tg + 1) * 512], in0=t1[tt][:], in1=t2[tt][:], op=ALU.add),
                                   reads=[b_t1[tt], b_t2[tt]], writes=[b_ostg[so]])
                        dst = QT if g < 4 else KT
                        S.dma("sp", lambda e, dst=dst, head=head, so=so: e.dma_start(out=dst[head], in_=ostg[so][:]), reads=[b_ostg[so]], writes=[S.dout("qk")], waw=False)
                else:
                    for c in range(4):
                        cc = (g - 8) * 4 + c
                        so = cc % 2
                        for tg in range(8):
                            k = mm_feat(u, c, tg)
                            if evi[0] % 2 == 0:
                                S.act(lambda e, k=k, so=so, tg=tg: e.copy(out=ostg[so][:, tg * 512:(tg + 1) * 512], in_=pf[k][:]), reads=[b_pf[k]], writes=[b_ostg[so]])
                            else:
                                S.dve(lambda e, k=k, so=so, tg=tg: e.tensor_copy(out=ostg[so][:, tg * 512:(tg + 1) * 512], in_=pf[k][:]), reads=[b_pf[k]], writes=[b_ostg[so]])
                            evi[0] += 1
                        S.dma("sp", lambda e, cc=cc, so=so: e.dma_start(out=XBCT[cc], in_=ostg[so][:]), reads=[b_ostg[so]], writes=[S.dout("xbc")], waw=False)
            tmaj = [(VT, 0, "copy"), (VT, 512, "copy"), (ZS, 0, "silu"), (ZS, 512, "silu"), (ZS, 1024, "silu"), (ZS, 1536, "silu"),
                    (GA, 0, "sig"), (GA, 512, "sig"), (GS, 0, "sig"), (GS, 512, "sig")]
            tsi = [0]
            for gi, (dst, c0, kind) in enumerate(tmaj):
                g = 16 + gi
                u = load_w(g)
                for i in range(NT):
                    k = pfi[0] % 4
                    pfi[0] += 1

                    def f(e, k=k, i=i, u=u):
                        r = None
                        for kc in range(8):
                            r = e.matmul(pf[k][:], lhsT=hT[:, kc, i * 128:(i + 1) * 128], rhs=wb[u][:, kc, :], start=(kc == 0), stop=(kc == 7))
                        return r
                    S.pe(f, reads=[b_wb[u], b_hT[i]], writes=[b_pf[k]])
                    ts_ = tsi[0] % 3
                    tsi[0] += 1
                    if kind == "copy":
                        S.dve(lambda e, k=k, ts_=ts_: e.tensor_copy(out=tstg[ts_][:], in_=pf[k][:]), reads=[b_pf[k]], writes=[b_tstg[ts_]])
                    else:
                        fn = AF.Silu if kind == "silu" else AF.Sigmoid
                        S.act(lambda e, k=k, ts_=ts_, fn=fn: e.activation(out=tstg[ts_][:], in_=pf[k][:], func=fn), reads=[b_pf[k]], writes=[b_tstg[ts_]])
                    S.dma("sp", lambda e, dst=dst, c0=c0, i=i, ts_=ts_: e.dma_start(out=dst[i * 128:(i + 1) * 128, c0:c0 + 512], in_=tstg[ts_][:]),
                          reads=[b_tstg[ts_]], writes=[S.dout("tm")], waw=False)
            u = load_w(26, ncols=32)
            for i in range(NT):
                k = pfi[0] % 4
                pfi[0] += 1

                def f(e, k=k, i=i, u=u):
                    r = None
                    for kc in range(8):
                        r = e.matmul(pf[k][:, 0:32], lhsT=hT[:, kc, i * 128:(i + 1) * 128], rhs=wb[u][:, kc, 0:32], start=(kc == 0), stop=(kc == 7))
                    return r
                S.pe(f, reads=[b_wb[u], b_hT[i]], writes=[b_pf[k]])
                ts_ = i % 2
                S.dve(lambda e, k=k, ts_=ts_: e.tensor_copy(out=tstf[ts_][:], in_=pf[k][:, 0:32]), reads=[b_pf[k]], writes=[b_tstf[ts_]])
                S.dma("sp", lambda e, i=i, ts_=ts_: e.dma_start(out=DTR[i * 128:(i + 1) * 128, :], in_=tstf[ts_][:]), reads=[b_tstf[ts_]], writes=[S.dout("dtr")], waw=False)
            S.flush()
        if stop_after <= 2:
            return nc, S

        lam_init = 0.8 - 0.6 * math.exp(-0.3 * 0)
        with contextlib.ExitStack() as st:
            def sb(name, shape, dt):
                return st.enter_context(nc.sbuf_tensor(name, list(shape), dt))

            def ps(name, shape, dt):
                return st.enter_context(nc.psum_tensor(name, list(shape), dt))

            lamv = sb("lamv", [128, 256], F32)
            lamw = sb("lamw", [128, 8], F32)
            lamj = sb("lamj", [128, 64], F32)
            sublnb = sb("sublnb", [128, 128], F32)
            b_lam = S.buf("lam")
            b_sub = S.buf("subln")
            S.dma("sp", lambda e: e.dma_start(out=lamv[:], in_=lam4.partition_broadcast(128)), writes=[b_lam])
            S.dma("sp", lambda e: e.dma_start(out=sublnb[:], in_=subln.partition_broadcast(128)), writes=[b_sub])

            S.seq("dve", [
                lambda e: e.tensor_tensor(out=lamj[:], in0=lamv[:, 0:64], in1=lamv[:, 64:128], op=ALU.mult),
                lambda e: e.tensor_reduce(out=lamw[:, 0:1], in_=lamj[:], axis=AX.X, op=ALU.add),
                lambda e: e.tensor_tensor(out=lamj[:], in0=lamv[:, 128:192], in1=lamv[:, 192:256], op=ALU.mult),
                lambda e: e.tensor_reduce(out=lamw[:, 1:2], in_=lamj[:], axis=AX.X, op=ALU.add),
            ], reads=[b_lam], writes=[b_lam])
            S.act(lambda e: e.activation(out=lamw[:, 2:4], in_=lamw[:, 0:2], func=AF.Exp), reads=[b_lam], writes=[b_lam])

            S.seq("dve", [
                lambda e: e.tensor_tensor(out=lamw[:, 4:5], in0=lamw[:, 3:4], in1=lamw[:, 2:3], op=ALU.subtract),
                lambda e: e.tensor_scalar(out=lamw[:, 5:6], in0=lamw[:, 4:5], scalar1=-lam_init, scalar2=None, op0=ALU.add),
            ], reads=[b_lam], writes=[b_lam])
            S.dve(lambda e: e.tensor_scalar(out=sublnb[:], in0=sublnb[:], scalar1=1.0 - lam_init, scalar2=None, op0=ALU.mult), reads=[b_sub], writes=[b_sub])

            qt = [sb(f"qt{i}", [128, 2048], BF16) for i in range(2)]
            kt = [sb(f"kt{i}", [128, 2048], BF16) for i in range(2)]
            vt = [sb(f"vt{i}", [128, 16, 130], BF16) for i in range(2)]
            b_qt = S.bufs(2, "qt")
            b_kt = S.bufs(2, "kt")
            b_vt = S.bufs(2, "vt")
            pT = [sb(f"pT{i}", [128, 512], BF16) for i in range(3)]
            b_pT = S.bufs(3, "pT")
            oc = [sb(f"oc{i}", [128, 16, 128], F32) for i in range(2)]
            b_oc = S.bufs(2, "oc")
            rcp = sb("rcp", [128, 64], F32)
            b_rcp = S.bufs(4, "rcp")
            dd = sb("dd", [128, 16, 128], F32)
            sq = sb("sq", [128, 16, 128], F32)
            sst = sb("sst", [128, 64], F32)
            b_dd = S.buf("dd")
            attb = [sb(f"attb{i}", [128, 16, 128], BF16) for i in range(2)]
            b_attb = S.bufs(2, "attb")
            pS = [ps(f"pS{i}", [128, 512], F32) for i in range(3)]
            b_pS = S.bufs(3, "pS")
            pO = [ps(f"pO{i}", [128, 512], F32) for i in range(4)]
            b_pO = S.bufs(4, "pO")
            for u in range(2):
                S.pool(lambda e, u=u: e.memset(vt[u][:, :, 128:129], 1.0), writes=[b_vt[u]])
            zfill = sb("zfill", [128, 8192], F32)
            b_zf = S.buf("zfill")
            S.pool(lambda e: e.memset(zfill[:], 0.0), writes=[b_zf])
            for zi in range(16):
                S.dma("sp", lambda e, zi=zi: e.dma_start(out=XBUF[zi * 1024:(zi + 1) * 1024, :].rearrange("(p j) d -> p (j d)", p=128), in_=zfill[:]),
                      reads=[b_zf], writes=[S.dout("xbuf0")], waw=False)
            si = [0]
            for s in range(2):
                for h in range(8):
                    u = (s * 8 + h) % 2
                    t0 = s * 2048
                    S.dma("sp", lambda e, u=u, h=h, t0=t0: e.dma_start(out=qt[u][:], in_=QT[h, :, t0:t0 + 2048]), writes=[b_qt[u]])
                    S.dma("sp", lambda e, u=u, h=h, t0=t0: e.dma_start(out=kt[u][:], in_=KT[h, :, t0:t0 + 2048]), writes=[b_kt[u]])
                    S.dma("sp", lambda e, u=u, h=h, t0=t0: e.dma_start(out=vt[u][:, :, 0:128],
                                                                          in_=VT[t0:t0 + 2048, h * 128:(h + 1) * 128].rearrange("(j p) e -> p j e", p=128)),
                          writes=[b_vt[u]])
                    for c in range(2):
                        for qc in range(4):
                            q_hi = (4 * qc + 4) * 128

                            def emit_S(j, u=u, c=c, qc=qc, q_hi=q_hi):
                                q_lo = max(j, 4 * qc) * 128
                                wd = q_hi - q_lo
                                r = si[0] % 3
                                si[0] += 1
                                S.pe(lambda e, r=r, u=u, c=c, j=j, q_lo=q_lo, q_hi=q_hi, wd=wd: e.matmul(
                                    pS[r][:, 0:wd], lhsT=kt[u][c * 64:(c + 1) * 64, j * 128:(j + 1) * 128],
                                    rhs=qt[u][c * 64:(c + 1) * 64, q_lo:q_hi], start=True, stop=True),
                                    reads=[b_kt[u], b_qt[u]], writes=[b_pS[r]])
                                return r
                            nj = 4 * qc + 4
                            r_next = emit_S(0)
                            for j in range(nj):
                                qb_lo = max(j, 4 * qc)
                                q_lo = qb_lo * 128
                                wd = q_hi - q_lo
                                r = r_next
                                if j + 1 < nj:
                                    r_next = emit_S(j + 1)
                                S.act(lambda e, r=r, wd=wd: e.activation(out=pT[r][:, 0:wd], in_=pS[r][:, 0:wd], func=AF.Exp, scale=0.125),
                                      reads=[b_pS[r]], writes=[b_pT[r]])
                                if j >= 4 * qc:
                                    S.pool(lambda e, r=r: e.affine_select(out=pT[r][:, 0:128], in_=pT[r][:, 0:128], pattern=[[1, 128]],
                                                                          compare_op=ALU.is_ge, fill=0.0, base=0, channel_multiplier=-1),
                                           reads=[b_pT[r]], writes=[b_pT[r]])
                                for i in range(qb_lo, 4 * qc + 4):
                                    ob = i - 4 * qc
                                    S.pe(lambda e, r=r, u=u, i=i, j=j, ob=ob, q_lo=q_lo: e.matmul(
                                        pO[ob][:, 0:129], lhsT=pT[r][:, i * 128 - q_lo:i * 128 - q_lo + 128], rhs=vt[u][:, j, 0:129],
                                        start=(j == 0), stop=(j == i)), reads=[b_pT[r], b_vt[u]], writes=[b_pO[ob]])
                            for ob in range(4):
                                i = 4 * qc + ob
                                S.dve(lambda e, ob=ob, i=i: e.reciprocal(out=rcp[:, ob * 16 + i:ob * 16 + i + 1], in_=pO[ob][:, 128:129]),
                                      reads=[b_pO[ob]], writes=[b_rcp[ob]])
                                S.dve(lambda e, ob=ob, i=i, c=c: e.tensor_scalar(out=oc[c][:, i, :], in0=pO[ob][:, 0:128],
                                                                                  scalar1=rcp[:, ob * 16 + i:ob * 16 + i + 1], scalar2=None, op0=ALU.mult),
                                      reads=[b_pO[ob], b_rcp[ob]], writes=[b_oc[c]])
                    S.dve(lambda e: e.scalar_tensor_tensor(out=dd[:], in0=oc[1][:], scalar=lamw[:, 5:6], in1=oc[0][:], op0=ALU.mult, op1=ALU.add),
                          reads=[b_oc[0], b_oc[1], b_lam], writes=[b_dd])
                    S.pool(lambda e: e.tensor_tensor(out=sq[:], in0=dd[:], in1=dd[:], op=ALU.mult), reads=[b_dd], writes=[b_dd])

                    S.seq("dve", [
                        lambda e: e.tensor_reduce(out=sst[:, 0:16], in_=sq[:], axis=AX.X, op=ALU.add),
                        lambda e: e.tensor_scalar(out=sst[:, 16:32], in0=sst[:, 0:16], scalar1=1.0 / 128, scalar2=1e-5, op0=ALU.mult, op1=ALU.add),
                    ], reads=[b_dd], writes=[b_dd])
                    S.act(lambda e: e.activation(out=sst[:, 32:48], in_=sst[:, 16:32], func=AF.Sqrt), reads=[b_dd], writes=[b_dd])
                    S.dve(lambda e: e.reciprocal(out=sst[:, 48:64], in_=sst[:, 32:48]), reads=[b_dd], writes=[b_dd])
                    S.dve(lambda e: e.tensor_tensor(out=dd[:], in0=dd[:], in1=bc3(sst[:, 48:64], 128), op=ALU.mult), reads=[b_dd], writes=[b_dd])
                    S.pool(lambda e, u=u: e.tensor_tensor(out=attb[u][:], in0=dd[:], in1=bcmid(sublnb[:, :], 16), op=ALU.mult),
                           reads=[b_dd, b_sub], writes=[b_attb[u]])
                    S.dma("sp", lambda e, u=u, h=h, t0=t0: e.dma_start(
                        out=ATT[t0:t0 + 2048, h * 128:(h + 1) * 128].rearrange("(j p) e -> p j e", p=128), in_=attb[u][:]),
                        reads=[b_attb[u]], writes=[S.dout("att")], waw=False)
            S.flush()
        if stop_after <= 3:
            return nc, S

        with contextlib.ExitStack() as st:
            def sb(name, shape, dt):
                return st.enter_context(nc.sbuf_tensor(name, list(shape), dt))

            def ps(name, shape, dt):
                return st.enter_context(nc.psum_tensor(name, list(shape), dt))

            cw = sb("cw", [128, 32, 4], F32)
            cb = sb("cb", [128, 32], F32)
            diag = sb("diag", [128, 32, 4, 128], BF16)
            b_cw = S.buf("cw")
            b_diag = S.buf("diag")
            vec3 = sb("vec3", [128, 96], F32)
            Abc = sb("Abc", [128, 32], F32)
            b_vec = S.buf("vec3")
            snw = sb("snw", [128, 2048], F32)
            b_snw = S.buf("snw")
            S.dma("sp", lambda e: e.dma_start(out=cw[:], in_=convw), writes=[b_cw])
            b_cb = S.buf("cb")
            S.dma("sp", lambda e: e.dma_start(out=cb[:], in_=convb), writes=[b_cb])
            S.dma("sp", lambda e: e.dma_start(out=vec3[:, 0:32], in_=dt_bias.partition_broadcast(128)), writes=[b_vec], waw=False)
            S.dma("sp", lambda e: e.dma_start(out=vec3[:, 32:64], in_=a_log.partition_broadcast(128)), writes=[b_vec], waw=False)
            S.dma("sp", lambda e: e.dma_start(out=vec3[:, 64:96], in_=d_skip.partition_broadcast(128)), writes=[b_vec], waw=False)
            S.dma("sp", lambda e: e.dma_start(out=snw[:], in_=ssd_nw.partition_broadcast(128)), writes=[b_snw])

            def mkdiag(e):
                r = None
                for cc in range(32):
                    for k in range(4):
                        r = e.tensor_scalar(out=diag[:, cc, k, :], in0=ident_f[:], scalar1=cw[:, cc, k:k + 1], scalar2=None, op0=ALU.mult)
                return r
            S.pool(mkdiag, reads=[b_cw], writes=[b_diag])
            S.act(lambda e: e.activation(out=Abc[:], in_=vec3[:, 32:64], func=AF.Exp), reads=[b_vec], writes=[b_vec])
            S.dve(lambda e: e.tensor_scalar(out=Abc[:], in0=Abc[:], scalar1=-1.0, scalar2=None, op0=ALU.mult), reads=[b_vec], writes=[b_vec])

            xr = [sb(f"xr{i}", [128, 32, 132], BF16) for i in range(2)]
            b_xrh = S.bufs(2, "xrh")
            b_xrA = S.bufs(2, "xrA")
            b_xrB = S.bufs(2, "xrB")
            import os as _os
            P4L = int(_os.environ.get("P4L", "99"))
            xc = [sb(f"xc{i}", [128, 32, 128], BF16) for i in range(2)]
            b_xc = S.bufs(2, "xc")
            xs_f_l = [sb(f"xs_f{i}", [128, 2048], F32) for i in range(2)]
            ztf = sb("ztf", [128, 2048], F32)
            b_ztf = S.buf("ztf")
            xdt_l = [sb(f"xdt{i}", [128, 2048], BF16) for i in range(2)]
            xdtw_l = [sb(f"xdtw{i}", [128, 2048], BF16) for i in range(2)]
            B_tok_l = [sb(f"B_tok{i}", [128, 8, 128], BF16) for i in range(2)]
            b_xs_l = S.bufs(2, "xs_f")
            b_xdt_l = S.bufs(2, "xdt")
            b_xdtw_l = S.bufs(2, "xdtw")
            b_Bt_l = S.bufs(2, "B_tok")
            dtt_l = [sb(f"dtt{i}", [128, 8, 32], F32) for i in range(2)]
            dtt2_l = [sb(f"dtt2{i}", [128, 3, 32], F32) for i in range(2)]
            b_dt_l = S.bufs(2, "dtt")
            E = [sb(f"E{i}", [128, 4, 128], F32) for i in range(2)]
            b_E = S.bufs(2, "E")
            M = [sb(f"M{i}", [128, 4, 128], BF16) for i in range(2)]
            b_M = S.bufs(2, "M")
            yoff = [sb(f"yoff{i}", [128, 256], F32) for i in range(2)]
            b_yoff = S.bufs(2, "yoff")
            y_l = [sb(f"y{i}", [128, 2048], F32) for i in range(2)]
            b_y_l = S.bufs(2, "y")
            ytmp_l = [sb(f"ytmp{i}", [128, 2048], F32) for i in range(2)]
            b_ytmp_l = S.bufs(2, "ytmp")
            zt = [sb(f"zt{i}", [128, 2048], BF16) for i in range(2)]
            b_zt = S.bufs(2, "zt")
            yo = [sb(f"yo{i}", [128, 2048], BF16) for i in range(2)]
            b_yo = S.bufs(2, "yo")
            gst_ = sb("gst", [128, 32], F32)
            Sst = sb("Sst", [128, 8, 256], F32)
            Sbf = sb("Sbf", [128, 8, 256], BF16)
            b_S = S.bufs(8, "Sst")
            b_Sbf = S.bufs(8, "Sbf")
            pcv = [ps(f"pcv{i}", [128, 4, 128], F32) for i in range(2)]
            b_pcv = S.bufs(2, "pcv")
            ptx = ps("ptx", [128, 16, 128], BF16)
            b_ptx = S.buf("ptx")
            pmisc = ps("pmisc", [128, 512], F32)
            b_pcm = S.buf("pcm")
            b_pcb = b_pcm
            b_pyo = b_pcm
            pseg = [ps(f"pseg{i}", [128, 4, 128], F32) for i in range(2)]
            b_pseg = S.bufs(2, "pseg")
            py = ps("py", [128, 512], F32)
            b_py = S.buf("py")
            b_pst = b_py
            ev = [0]
            pending = [None]
            for s in range(2 if P4L >= 99 else 1):
                for ch in range(16 if P4L >= 99 else 2):
                    def tile_body(s=s, ch=ch):
                        ti = s * 16 + ch
                        u = ti % 2
                        xs_f, xdt, xdtw, B_tok, dtt, dtt2, y, ytmp = xs_f_l[u], xdt_l[u], xdtw_l[u], B_tok_l[u], dtt_l[u], dtt2_l[u], y_l[u], ytmp_l[u]
                        b_xs, b_xdt, b_xdtw, b_Bt, b_dt, b_y, b_ytmp = b_xs_l[u], b_xdt_l[u], b_xdtw_l[u], b_Bt_l[u], b_dt_l[u], b_y_l[u], b_ytmp_l[u]
                        ti = s * 16 + ch
                        t0 = ti * 128
                        u = ti % 2
                        if ch == 0:
                            S.pool(lambda e, u=u: e.memset(xr[u][:, :, 0:4], 0.0), writes=[b_xrh[u]])
                            S.pool(lambda e: e.memset(Sst[:], 0.0), writes=b_S)
                            S.pool(lambda e: e.memset(Sbf[:], 0.0), writes=b_Sbf)
                        else:
                            S.act(lambda e, u=u: e.copy(out=xr[u][:, :, 1:4], in_=xr[1 - u][:, :, 129:132]),
                                   reads=[b_xrA[1 - u], b_xrB[1 - u]], writes=[b_xrh[u]])
                        S.dma("sp", lambda e, u=u, t0=t0: e.dma_start(out=xr[u][:, 0:16, 4:132], in_=XBCT[0:16, :, t0:t0 + 128].rearrange("c p t -> p c t")),
                              writes=[b_xrA[u]])
                        S.dma("sp", lambda e, u=u, t0=t0: e.dma_start(out=xr[u][:, 16:32, 4:132], in_=XBCT[16:32, :, t0:t0 + 128].rearrange("c p t -> p c t")),
                              writes=[b_xrB[u]])
                        S.dma("sp", lambda e, t0=t0: e.dma_start(out=dtt[:, 0, :], in_=DTR[t0:t0 + 128, :]), writes=[b_dt])
                        S.dma("sp", lambda e, u=u, t0=t0: e.dma_start(out=zt[u][:], in_=ZS[t0:t0 + 128, :]), writes=[b_zt[u]])
                        if P4L <= 1:
                            return
                        S.dve(lambda e: e.tensor_tensor(out=dtt[:, 1, :], in0=dtt[:, 0, :], in1=vec3[:, 0:32], op=ALU.add), reads=[b_dt, b_vec], writes=[b_dt])
                        S.act(lambda e: e.activation(out=dtt[:, 2, :], in_=dtt[:, 1, :], func=AF.Exp), reads=[b_dt], writes=[b_dt])
                        S.act(lambda e: e.activation(out=dtt[:, 3, :], in_=dtt[:, 2, :], func=AF.Ln, bias=1.0, scale=1.0), reads=[b_dt], writes=[b_dt])
                        S.dve(lambda e: e.tensor_tensor(out=dtt[:, 4, :], in0=dtt[:, 3, :], in1=Abc[:], op=ALU.mult), reads=[b_dt, b_vec], writes=[b_dt])

                        def cumf(e):
                            e.matmul(pmisc[:, 0:32], lhsT=tri_f[:], rhs=dtt[:, 4, :], start=True, stop=True)
                            return e.matmul(pmisc[:, 32:64], lhsT=ones_f[:], rhs=dtt[:, 4, :], start=True, stop=True)
                        S.pe(cumf, reads=[b_dt], writes=[b_pcm])
                        S.dve(lambda e: e.tensor_scalar(out=dtt[:, 5, :], in0=pmisc[:, 0:32], scalar1=-1.0, scalar2=None, op0=ALU.mult), reads=[b_pcm], writes=[b_dt])
                        S.dve(lambda e: e.tensor_tensor(out=dtt2[:, 1, :], in0=pmisc[:, 32:64], in1=dtt[:, 5, :], op=ALU.add), reads=[b_pcm, b_dt], writes=[b_dt])

                        def expf(e):
                            e.activation(out=dtt[:, 6, :], in_=pmisc[:, 0:32], func=AF.Exp)
                            e.activation(out=dtt2[:, 0, :], in_=pmisc[:, 32:64], func=AF.Exp)
                            return e.activation(out=dtt[:, 7, :], in_=dtt2[:, 1, :], func=AF.Exp)
                        S.act(expf, reads=[b_pcm, b_dt], writes=[b_dt])
                        if P4L <= 2:
                            return
                        for c4 in range(8):
                            pv = c4 % 2

                            def convf(e, c4=c4, pv=pv, u=u):
                                r = None
                                for q in range(4):
                                    cc = c4 * 4 + q
                                    for k in range(4):
                                        r = e.matmul(pcv[pv][:, q, :], lhsT=diag[:, cc, k, :], rhs=xr[u][:, cc, k + 1:k + 129], start=(k == 0), stop=(k == 3))
                                return r
                            S.pe(convf, reads=[b_diag, b_xrh[u], b_xrA[u], b_xrB[u]], writes=[b_pcv[pv]])

                            def siluf(e, c4=c4, pv=pv, u=u):
                                r = None
                                for q in range(4):
                                    cc = c4 * 4 + q
                                    r = e.activation(out=xc[u][:, cc, :], in_=pcv[pv][:, q, :], func=AF.Silu, bias=cb[:, cc:cc + 1], scale=1.0)
                                return r
                            S.act(siluf, reads=[b_pcv[pv], b_cb], writes=[b_xc[u]])
                        if P4L <= 3:
                            return

                        def tpf(e, u=u):
                            r = None
                            for cc in range(16):
                                r = e.transpose(out=ptx[:, cc, :], in_=xc[u][:, cc, :], identity=ident_b[:])
                            return r
                        S.pe(tpf, reads=[b_xc[u]], writes=[b_ptx])
                        def xscp(e):
                            e.copy(out=xs_f[:, 0:1024].rearrange("p (c t) -> p c t", t=128), in_=ptx[:, 0:8, :])
                            return e.copy(out=xs_f[:, 1024:2048].rearrange("p (c t) -> p c t", t=128), in_=ptx[:, 8:16, :])
                        S.act(xscp, reads=[b_ptx], writes=[b_xs])

                        def tpb(e, u=u):
                            r = None
                            for cc in range(8):
                                r = e.transpose(out=ptx[:, cc, :], in_=xc[u][:, 16 + cc, :], identity=ident_b[:])
                            return r
                        S.pe(tpb, reads=[b_xc[u]], writes=[b_ptx])
                        S.act(lambda e: e.copy(out=B_tok[:], in_=ptx[:, 0:8, :]), reads=[b_ptx], writes=[b_Bt])
                        S.dve(lambda e: e.tensor_tensor(out=dtt2[:, 2, :], in0=dtt[:, 3, :], in1=dtt[:, 7, :], op=ALU.mult), reads=[b_dt], writes=[b_dt])
                        S.dve(lambda e: e.tensor_tensor(out=xdt[:].rearrange("p (h d) -> p h d", d=64), in0=xs_f[:].rearrange("p (h d) -> p h d", d=64),
                                                        in1=bc3(dtt[:, 3, :], 64), op=ALU.mult), reads=[b_xs, b_dt], writes=[b_xdt])
                        S.pool(lambda e: e.tensor_tensor(out=xdtw[:].rearrange("p (h d) -> p h d", d=64), in0=xs_f[:].rearrange("p (h d) -> p h d", d=64),
                                                         in1=bc3(dtt2[:, 2, :], 64), op=ALU.mult), reads=[b_xs, b_dt], writes=[b_xdtw])
                        if P4L <= 4:
                            return
                        def emit_seg(g, ti=ti):
                            eu = (ti * 8 + g) % 2

                            def segf(e, g=g, eu=eu):
                                r = None
                                for r_ in range(4):
                                    hh = 4 * g + r_
                                    e.matmul(pseg[eu][:, r_, :], lhsT=dtt[:, 4, hh:hh + 1].to_broadcast([128, 128]), rhs=tri_f[:], start=True, stop=False)
                                    r = e.matmul(pseg[eu][:, r_, :], lhsT=ident_f[:], rhs=negmask[:], start=False, stop=True)
                                return r
                            S.pe(segf, reads=[b_dt], writes=[b_pseg[eu]])

                            def expg(e, g=g, eu=eu):
                                r = None
                                for r_ in range(4):
                                    hh = 4 * g + r_
                                    r = e.activation(out=E[eu][:, r_, :], in_=pseg[eu][:, r_, :], func=AF.Exp, bias=dtt[:, 5, hh:hh + 1], scale=1.0)
                                return r
                            S.act(expg, reads=[b_pseg[eu], b_dt], writes=[b_E[eu]])
                        emit_seg(0)
                        for g in range(8):
                            eu = (ti * 8 + g) % 2
                            S.pe(lambda e, g=g, u=u: e.matmul(pmisc[:, 128:256], lhsT=xc[u][:, 16 + g, :], rhs=xc[u][:, 24 + g, :], start=True, stop=True),
                                 reads=[b_xc[u]], writes=[b_pcb])
                            S.dve(lambda e, eu=eu: e.tensor_tensor(out=M[eu][:], in0=E[eu][:], in1=bcmid(pmisc[:, 128:256], 4), op=ALU.mult),
                                  reads=[b_E[eu], b_pcb], writes=[b_M[eu]])
                            if g + 1 < 8:
                                emit_seg(g + 1)

                            def ydf(e, g=g, eu=eu):
                                r = None
                                for r_ in range(4):
                                    hh = 4 * g + r_
                                    r = e.matmul(py[:, r_ * 64:(r_ + 1) * 64], lhsT=M[eu][:, r_, :], rhs=xdt[:, hh * 64:(hh + 1) * 64], start=True, stop=True)
                                return r
                            S.pe(ydf, reads=[b_M[eu], b_xdt], writes=[b_py])
                            S.pe(lambda e, g=g, u=u: e.matmul(pmisc[:, 256:512], lhsT=xc[u][:, 24 + g, :], rhs=Sbf[:, g, :], start=True, stop=True),
                                 reads=[b_xc[u], b_Sbf[g]], writes=[b_pyo])

                            def yoffs(e, g=g, eu=eu):
                                r = None
                                for r_ in range(4):
                                    hh = 4 * g + r_
                                    r = e.activation(out=yoff[eu][:, r_ * 64:(r_ + 1) * 64], in_=pmisc[:, 256 + r_ * 64:256 + (r_ + 1) * 64], func=AF.Copy,
                                                     scale=dtt[:, 6, hh:hh + 1])
                                return r
                            S.act(yoffs, reads=[b_pyo, b_dt], writes=[b_yoff[eu]])
                            S.dve(lambda e, g=g, eu=eu: e.tensor_tensor(out=y[:, g * 256:(g + 1) * 256], in0=py[:, 0:256], in1=yoff[eu][:], op=ALU.add),
                                  reads=[b_py, b_yoff[eu]], writes=[b_y])
                            S.pe(lambda e, g=g: e.matmul(py[:, 256:512], lhsT=B_tok[:, g, :], rhs=xdtw[:, g * 256:(g + 1) * 256], start=True, stop=True),
                                 reads=[b_Bt, b_xdtw], writes=[b_pst])
                            S.pool(lambda e, g=g: e.tensor_tensor(out=Sst[:, g, :].rearrange("p (r d) -> p r d", d=64), in0=Sst[:, g, :].rearrange("p (r d) -> p r d", d=64),
                                                                  in1=bc3(dtt2[:, 0, 4 * g:4 * g + 4], 64), op=ALU.mult), reads=[b_dt, b_Sbf[g]], writes=[b_S[g]])
                            S.dve(lambda e, g=g: e.tensor_tensor(out=Sst[:, g, :], in0=py[:, 256:512], in1=Sst[:, g, :], op=ALU.add), reads=[b_pst, b_S[g]], writes=[b_S[g]])
                            S.pool(lambda e, g=g: e.tensor_copy(out=Sbf[:, g, :], in_=Sst[:, g, :]), reads=[b_S[g]], writes=[b_Sbf[g]])
                        if P4L <= 5:
                            return
                        yield
                        S.act(lambda e, u=u: e.copy(out=ztf[:], in_=zt[u][:]), reads=[b_zt[u]], writes=[b_ztf])
                        S.pool(lambda e: e.tensor_tensor(out=ytmp[:].rearrange("p (h d) -> p h d", d=64), in0=xs_f[:].rearrange("p (h d) -> p h d", d=64),
                                                         in1=bc3(vec3[:, 64:96], 64), op=ALU.mult), reads=[b_xs, b_vec], writes=[b_ytmp])
                        S.dve(lambda e: e.tensor_tensor(out=y[:], in0=y[:], in1=ytmp[:], op=ALU.add), reads=[b_y, b_ytmp], writes=[b_y])
                        S.dve(lambda e: e.tensor_tensor(out=y[:], in0=y[:], in1=ztf[:], op=ALU.mult), reads=[b_y, b_ztf], writes=[b_y])
                        S.act(lambda e: e.activation(out=ytmp[:], in_=y[:], func=AF.Square), reads=[b_y], writes=[b_ytmp])

                        S.seq("dve", [
                            lambda e: e.tensor_reduce(out=gst_[:, 0:8], in_=ytmp[:].rearrange("p (g d) -> p g d", d=256), axis=AX.X, op=ALU.add),
                            lambda e: e.tensor_scalar(out=gst_[:, 8:16], in0=gst_[:, 0:8], scalar1=1.0 / 256, scalar2=1e-5, op0=ALU.mult, op1=ALU.add),
                        ], reads=[b_ytmp], writes=[b_ytmp])
                        S.act(lambda e: e.activation(out=gst_[:, 16:24], in_=gst_[:, 8:16], func=AF.Sqrt), reads=[b_ytmp], writes=[b_ytmp])
                        S.dve(lambda e: e.reciprocal(out=gst_[:, 24:32], in_=gst_[:, 16:24]), reads=[b_ytmp], writes=[b_ytmp])
                        S.dve(lambda e: e.tensor_tensor(out=y[:].rearrange("p (g d) -> p g d", d=256), in0=y[:].rearrange("p (g d) -> p g d", d=256),
                                                        in1=bc3(gst_[:, 24:32], 256), op=ALU.mult), reads=[b_y, b_ytmp], writes=[b_y])
                        S.dve(lambda e, u=u: e.tensor_tensor(out=yo[u][:], in0=y[:], in1=snw[:], op=ALU.mult), reads=[b_y, b_snw], writes=[b_yo[u]])
                        S.dma("sp", lambda e, u=u, t0=t0: e.dma_start(out=SSD[t0:t0 + 128, :], in_=yo[u][:]), reads=[b_yo[u]], writes=[S.dout("ssd")], waw=False)
                    gen = tile_body()
                    try:
                        next(gen)
                    except StopIteration:
                        gen = None
                    if pending[0] is not None:
                        for _ in pending[0]:
                            pass
                    pending[0] = gen
            if pending[0] is not None:
                for _ in pending[0]:
                    pass
            S.flush()
        if stop_after <= 4:
            return nc, S

        with contextlib.ExitStack() as st:
            def sb(name, shape, dt):
                return st.enter_context(nc.sbuf_tensor(name, list(shape), dt))

            def ps(name, shape, dt):
                return st.enter_context(nc.psum_tensor(name, list(shape), dt))

            LG = sb("LG", [128, NT, 72], F32)
            W1 = sb("W1", [128, NT], F32)
            W2 = sb("W2", [128, NT], F32)
            destI = sb("destI", [128, 2, NT], I32)
            IDX = sb("IDX", [128, 128], I32)
            b_LG = S.bufs(NT, "LG")
            with contextlib.ExitStack() as st5:
                def sb5(name, shape, dt):
                    return st5.enter_context(nc.sbuf_tensor(name, list(shape), dt))

                def ps5(name, shape, dt):
                    return st5.enter_context(nc.psum_tensor(name, list(shape), dt))
                Pa = sb5("Pa", [128, 8, 1024], BF16)
                Ps = sb5("Ps", [128, 16, 1024], BF16)
                Wo = sb5("Wo", [128, 8, 1024], BF16)
                Wr = sb5("Wr", [128, 8, 72], F32)
                brb = sb5("brb", [128, 72], F32)
                nw2 = sb5("nw2", [128, 1024], F32)
                b_W = S.buf("W5")
                wst = [sb5(f"wst{i}", [128, 8, 512], F32) for i in range(2)]
                b_wst = S.bufs(2, "wst")
                b_small = S.bufs(3, "small")
                S.dma("sp", lambda e: e.dma_start(out=Wr[:], in_=w_r.rearrange("(kc p) n -> p kc n", p=128)), writes=[b_small[0]])
                S.dma("sp", lambda e: e.dma_start(out=brb[:], in_=b_r.partition_broadcast(128)), writes=[b_small[1]])
                S.dma("sp", lambda e: e.dma_start(out=nw2[:], in_=normw2.partition_broadcast(128)), writes=[b_small[2]])
                wi = 0
                for (src, dstt, nk) in ((w_pa, Pa, 8), (w_ps, Ps, 16), (w_o, Wo, 8)):
                    sv = src.rearrange("(kc p) n -> p kc n", p=128)
                    for k0 in range(0, nk, 8):
                        for half in range(2):
                            u = wi % 2
                            wi += 1
                            S.dma("sp", lambda e, u=u, sv=sv, k0=k0, half=half: e.dma_start(out=wst[u][:], in_=sv[:, k0:k0 + 8, half * 512:(half + 1) * 512]),
                                  writes=[b_wst[u]])
                            S.pool(lambda e, u=u, dstt=dstt, k0=k0, half=half: e.tensor_copy(out=dstt[:, k0:k0 + 8, half * 512:(half + 1) * 512], in_=wst[u][:]),
                                   reads=[b_wst[u]], writes=[b_W])
                at = [sb5(f"at{i}", [128, 1024], BF16) for i in range(2)]
                sdt = [sb5(f"sdt{i}", [128, 2048], BF16) for i in range(2)]
                gat_ = [sb5(f"ga{i}", [128, 1024], BF16) for i in range(2)]
                gst2 = [sb5(f"gs{i}", [128, 1024], BF16) for i in range(2)]
                xt5 = [sb5(f"xt5_{i}", [128, 1024], F32) for i in range(2)]
                b_in5 = S.bufs(2, "in5")
                gaf = sb5("gaf", [128, 1024], F32)
                gsf = sb5("gsf", [128, 1024], F32)
                b_gf = S.buf("gf")
                aT = sb5("aT", [128, 8, 128], BF16)
                sT = sb5("sT", [128, 16, 128], BF16)
                mT = sb5("mT", [128, 8, 128], BF16)
                b_aT = S.buf("aT")
                b_sT = S.buf("sT")
                b_mT = S.buf("mT")
                m1 = sb5("m1", [128, 1024], F32)
                m2 = sb5("m2", [128, 1024], F32)
                mg = sb5("mg", [128, 1024], BF16)
                b_m1 = S.buf("m1")
                b_m2 = S.buf("m2")
                b_mg = S.buf("mg")
                x1t = [sb5(f"x1t{i}", [128, 1024], F32) for i in range(2)]
                b_x1t = S.bufs(2, "x1t")
                h2t = [sb5(f"h2t{i}", [128, 1024], F32) for i in range(2)]
                b_h2t = S.bufs(2, "h2t")
                h2T = sb5("h2T", [128, 8, 128], F32)
                b_h2T = S.buf("h2T")
                junk5 = sb5("junk5", [128, 1024], BF16)
                b_junk5 = S.buf("junk5")
                st5s = sb5("st5s", [128, NT, 4], F32)
                b_st5 = S.bufs(NT, "st5")
                pta = ps5("pta", [128, 8, 128], BF16)
                pts = ps5("pts", [128, 16, 128], BF16)
                b_pta = S.buf("pta")
                b_pts = S.buf("pts")
                pp = [ps5(f"pp{i}", [128, 512], F32) for i in range(3)]
                b_pp = S.bufs(3, "pp")
                ph = ps5("ph", [128, 8, 128], F32)
                b_ph = S.buf("ph")
                ppi = [0]

                def proj(lhs, bl, W, nk, half):
                    k = ppi[0] % 3
                    ppi[0] += 1

                    def f(e):
                        r = None
                        for kc in range(nk):
                            r = e.matmul(pp[k][:], lhsT=lhs[:, kc, :], rhs=W[:, kc, half * 512:(half + 1) * 512], start=(kc == 0), stop=(kc == nk - 1))
                        return r
                    S.pe(f, reads=[bl, b_W], writes=[b_pp[k]])
                    return k

                for i in range(NT):
                    u = i % 2
                    r0 = i * 128
                    S.dma("sp", lambda e, u=u, r0=r0: e.dma_start(out=at[u][:], in_=ATT[r0:r0 + 128, :]), writes=[b_in5[u]])
                    S.dma("sp", lambda e, u=u, r0=r0: e.dma_start(out=sdt[u][:], in_=SSD[r0:r0 + 128, :]), writes=[b_in5[u]], waw=False)
                    S.dma("sp", lambda e, u=u, r0=r0: e.dma_start(out=gat_[u][:], in_=GA[r0:r0 + 128, :]), writes=[b_in5[u]], waw=False)
                    S.dma("sp", lambda e, u=u, r0=r0: e.dma_start(out=gst2[u][:], in_=GS[r0:r0 + 128, :]), writes=[b_in5[u]], waw=False)
                    S.dma("sp", lambda e, u=u, r0=r0: e.dma_start(out=xt5[u][:], in_=x[r0:r0 + 128, :]), writes=[b_in5[u]], waw=False)

                    def tpa(e, u=u):
                        r = None
                        for kc in range(8):
                            r = e.transpose(out=pta[:, kc, :], in_=at[u][:, kc * 128:(kc + 1) * 128], identity=ident_b[:])
                        return r
                    S.pe(tpa, reads=[b_in5[u]], writes=[b_pta])
                    S.act(lambda e: e.copy(out=aT[:], in_=pta[:]), reads=[b_pta], writes=[b_aT])

                    def tps(e, u=u):
                        r = None
                        for kc in range(16):
                            r = e.transpose(out=pts[:, kc, :], in_=sdt[u][:, kc * 128:(kc + 1) * 128], identity=ident_b[:])
                        return r
                    S.pe(tps, reads=[b_in5[u]], writes=[b_pts])
                    def stcp(e):
                        e.copy(out=sT[:, 0:8, :], in_=pts[:, 0:8, :])
                        return e.copy(out=sT[:, 8:16, :], in_=pts[:, 8:16, :])
                    S.act(stcp, reads=[b_pts], writes=[b_sT])

                    def gcv(e, u=u):
                        e.tensor_copy(out=gaf[:], in_=gat_[u][:])
                        return e.tensor_copy(out=gsf[:], in_=gst2[u][:])
                    S.pool(gcv, reads=[b_in5[u]], writes=[b_gf])
                    for half in range(2):
                        hs = slice(half * 512, (half + 1) * 512)
                        ka = proj(aT, b_aT, Pa, 8, half)
                        S.dve(lambda e, ka=ka, hs=hs, u=u: e.tensor_tensor(out=m1[:, hs], in0=pp[ka][:], in1=gaf[:, hs], op=ALU.mult),
                              reads=[b_pp[ka], b_gf], writes=[b_m1])
                        ks = proj(sT, b_sT, Ps, 16, half)
                        S.dve(lambda e, ks=ks, hs=hs, u=u: e.tensor_tensor(out=m2[:, hs], in0=pp[ks][:], in1=gsf[:, hs], op=ALU.mult),
                              reads=[b_pp[ks], b_gf], writes=[b_m2])
                    S.pool(lambda e: e.tensor_tensor(out=mg[:], in0=m1[:], in1=m2[:], op=ALU.add), reads=[b_m1, b_m2], writes=[b_mg])

                    def tpm(e):
                        r = None
                        for kc in range(8):
                            r = e.transpose(out=pta[:, kc, :], in_=mg[:, kc * 128:(kc + 1) * 128], identity=ident_b[:])
                        return r
                    S.pe(tpm, reads=[b_mg], writes=[b_pta])
                    S.act(lambda e: e.copy(out=mT[:], in_=pta[:]), reads=[b_pta], writes=[b_mT])
                    for half in range(2):
                        hs = slice(half * 512, (half + 1) * 512)
                        ko = proj(mT, b_mT, Wo, 8, half)
                        S.dve(lambda e, ko=ko, hs=hs, u=u: e.tensor_tensor(out=x1t[u][:, hs], in0=pp[ko][:], in1=xt5[u][:, hs], op=ALU.add),
                              reads=[b_pp[ko], b_in5[u]], writes=[b_x1t[u]])
                    S.dma("sp", lambda e, u=u, r0=r0: e.dma_start(out=X1[r0:r0 + 128, :], in_=x1t[u][:]), reads=[b_x1t[u]], writes=[S.dout("x1")], waw=False)
                    S.act(lambda e, i=i, u=u: e.activation(out=junk5[:], in_=x1t[u][:], func=AF.Square, accum_out=st5s[:, i, 0:1]),
                          reads=[b_x1t[u]], writes=[b_junk5, b_st5[i]])
                    S.dve(lambda e, i=i: e.tensor_scalar(out=st5s[:, i, 1:2], in0=st5s[:, i, 0:1], scalar1=1.0 / D, scalar2=1e-6, op0=ALU.mult, op1=ALU.add),
                          reads=[b_st5[i]], writes=[b_st5[i]])
                    S.act(lambda e, i=i: e.activation(out=st5s[:, i, 2:3], in_=st5s[:, i, 1:2], func=AF.Sqrt), reads=[b_st5[i]], writes=[b_st5[i]])
                    S.dve(lambda e, i=i: e.reciprocal(out=st5s[:, i, 3:4], in_=st5s[:, i, 2:3]), reads=[b_st5[i]], writes=[b_st5[i]])
                    S.dve(lambda e, i=i, u=u: e.tensor_scalar(out=h2t[u][:], in0=x1t[u][:], scalar1=st5s[:, i, 3:4], scalar2=None, op0=ALU.mult),
                          reads=[b_x1t[u], b_st5[i]], writes=[b_h2t[u]])
                    S.pool(lambda e, u=u: e.tensor_tensor(out=h2t[u][:], in0=h2t[u][:], in1=nw2[:], op=ALU.mult), reads=[b_h2t[u]] + b_small, writes=[b_h2t[u]])
                    S.dma("sp", lambda e, u=u, r0=r0: e.dma_start(out=H2[r0:r0 + 128, :], in_=h2t[u][:]), reads=[b_h2t[u]], writes=[S.dout("h2")], waw=False)

                    def tph(e, u=u):
                        r = None
                        for kc in range(8):
                            r = e.transpose(out=ph[:, kc, :], in_=h2t[u][:, kc * 128:(kc + 1) * 128], identity=ident_f[:])
                        return r
                    S.pe(tph, reads=[b_h2t[u]], writes=[b_ph])
                    def h2cp(e):
                        e.copy(out=h2T[:, 0:4, :], in_=ph[:, 0:4, :])
                        return e.copy(out=h2T[:, 4:8, :], in_=ph[:, 4:8, :])
                    S.act(h2cp, reads=[b_ph], writes=[b_h2T])
                    k = ppi[0] % 3
                    ppi[0] += 1

                    def rmm(e, k=k):
                        r = None
                        for kc in range(8):
                            r = e.matmul(pp[k][:, 0:72], lhsT=h2T[:, kc, :], rhs=Wr[:, kc, :], start=(kc == 0), stop=(kc == 7))
                        return r
                    S.pe(rmm, reads=[b_h2T] + b_small, writes=[b_pp[k]])
                    S.dve(lambda e, k=k, i=i: e.tensor_tensor(out=LG[:, i, :], in0=pp[k][:, 0:72], in1=brb[:], op=ALU.add),
                          reads=[b_pp[k]] + b_small, writes=[b_LG[i]])
                S.flush()
            if stop_after <= 5:
                return nc, S

            with contextlib.ExitStack() as st6:
                def sb6(name, shape, dt):
                    return st6.enter_context(nc.sbuf_tensor(name, list(shape), dt))

                def ps6(name, shape, dt):
                    return st6.enter_context(nc.psum_tensor(name, list(shape), dt))
                R = S.buf("R")
                gmax = sb6("gmax", [128, NT], F32)
                goh = sb6("goh", [128, NT, 8], F32)
                gex = sb6("gex", [128, NT, 8], F32)
                gsum = sb6("gsum", [128, NT], F32)
                ggate = sb6("ggate", [128, NT], F32)
                tmp64 = sb6("tmp64", [128, NT, 8, 8], F32)
                esel = sb6("esel", [128, NT, 8], F32)
                e2 = sb6("e2", [128, NT, 8], F32)
                m1_ = sb6("m1_", [128, NT], F32)
                m2_ = sb6("m2_", [128, NT], F32)
                oh1 = sb6("oh1", [128, NT, 8], F32)
                oh2 = sb6("oh2", [128, NT, 8], F32)
                dd_ = sb6("dd_", [128, NT], F32)
                A0 = sb6("A0", [128, NT, 64], F32)
                A1 = sb6("A1", [128, NT, 64], F32)
                Ab = sb6("Ab", [128, NT, 64], BF16)
                pref = sb6("pref", [128, NT, 64], F32)
                cnt = sb6("cnt", [128, 64], F32)
                padded = sb6("padded", [128, 64], F32)
                cntI = sb6("cntI", [128, 64], I32)
                pend = sb6("pend", [128, 64], F32)
                pstart = sb6("pstart", [128, 64], F32)
                dest = sb6("dest", [128, 2, NT], F32)
                bval = sb6("bval", [128, 128], F32)
                bvalI = sb6("bvalI", [128, 128], I32)
                pidxI = sb6("pidxI", [128, 1], I32)
                pidx = sb6("pidx", [128, 1], F32)
                cmp = sb6("cmp", [128, 128, 64], F32)
                blkE = sb6("blkE", [128, 128], F32)
                IDXf = sb6("IDXf", [128, 128], F32)
                ppre = [ps6(f"ppre{i}", [128, 512], F32) for i in range(2)]
                b_ppre = S.bufs(2, "ppre")
                pcnt = ps6("pcnt", [128, 512], F32)
                b_pcnt = S.buf("pcnt")
                LGg = LG[:, :, 0:8]
                LGe = LG[:, :, 8:72].rearrange("p t (g e) -> p t g e", e=8)

                S.seq("dve", [
                    lambda e: e.tensor_reduce(out=gmax[:], in_=LGg, axis=AX.X, op=ALU.max),
                    lambda e: e.tensor_tensor(out=goh[:], in0=LGg, in1=bc3(gmax[:, :], 8), op=ALU.is_equal),
                    lambda e: e.tensor_tensor(out=gex[:], in0=LGg, in1=bc3(gmax[:, :], 8), op=ALU.subtract),
                ], reads=b_LG, writes=[R])
                S.act(lambda e: e.activation(out=gex[:], in_=gex[:], func=AF.Exp), reads=[R], writes=[R])

                S.seq("dve", [
                    lambda e: e.tensor_reduce(out=gsum[:], in_=gex[:], axis=AX.X, op=ALU.add),
                    lambda e: e.reciprocal(out=ggate[:], in_=gsum[:]),
                    lambda e: e.tensor_tensor(out=tmp64[:], in0=LGe, in1=goh[:].unsqueeze(3).to_broadcast([128, NT, 8, 8]), op=ALU.mult),
                    lambda e: e.tensor_reduce(out=esel[:], in_=tmp64[:].rearrange("p t g e -> p t e g"), axis=AX.X, op=ALU.add),
                    lambda e: e.tensor_reduce(out=m1_[:], in_=esel[:], axis=AX.X, op=ALU.max),
                    lambda e: e.tensor_tensor(out=oh1[:], in0=esel[:], in1=bc3(m1_[:, :], 8), op=ALU.is_equal),
                    lambda e: e.scalar_tensor_tensor(out=e2[:], in0=oh1[:], scalar=-1e30, in1=esel[:], op0=ALU.mult, op1=ALU.add),
                    lambda e: e.tensor_reduce(out=m2_[:], in_=e2[:], axis=AX.X, op=ALU.max),
                    lambda e: e.tensor_tensor(out=oh2[:], in0=e2[:], in1=bc3(m2_[:, :], 8), op=ALU.is_equal),
                    lambda e: e.tensor_tensor(out=dd_[:], in0=m2_[:], in1=m1_[:], op=ALU.subtract),
                ], reads=[R] + b_LG, writes=[R])
                S.act(lambda e: e.activation(out=dd_[:], in_=dd_[:], func=AF.Exp), reads=[R], writes=[R])

                S.seq("dve", [
                    lambda e: e.tensor_scalar(out=dd_[:], in0=dd_[:], scalar1=1.0, scalar2=None, op0=ALU.add),
                    lambda e: e.reciprocal(out=W1[:], in_=dd_[:]),
                    lambda e: e.tensor_scalar(out=W2[:], in0=W1[:], scalar1=-1.0, scalar2=1.0, op0=ALU.mult, op1=ALU.add),
                    lambda e: e.tensor_tensor(out=W1[:], in0=W1[:], in1=ggate[:], op=ALU.mult),
                    lambda e: e.tensor_tensor(out=W2[:], in0=W2[:], in1=ggate[:], op=ALU.mult),
                    lambda e: e.tensor_tensor(out=A0[:].rearrange("p t (g e) -> p t g e", e=8), in0=goh[:].unsqueeze(3).to_broadcast([128, NT, 8, 8]), in1=oh1[:].unsqueeze(2).to_broadcast([128, NT, 8, 8]), op=ALU.mult),
                    lambda e: e.tensor_tensor(out=A1[:].rearrange("p t (g e) -> p t g e", e=8), in0=goh[:].unsqueeze(3).to_broadcast([128, NT, 8, 8]), in1=oh2[:].unsqueeze(2).to_broadcast([128, NT, 8, 8]), op=ALU.mult),
                    lambda e: e.tensor_tensor(out=Ab[:], in0=A0[:], in1=A1[:], op=ALU.add),
                ], reads=[R], writes=[R])

                def cntf(e):
                    r = None
                    for T in range(NT):
                        r = e.matmul(pcnt[:, 0:64], lhsT=ones_b[:], rhs=Ab[:, T, :], start=(T == 0), stop=(T == NT - 1))
                    return r
                S.pe(cntf, reads=[R], writes=[b_pcnt])
                prefbufs = S.bufs(NT, "pref")
                for T in range(NT):
                    k = T % 2

                    def pref_f(e, T=T, k=k):
                        r = e.matmul(ppre[k][:, 0:64], lhsT=striu_b[:], rhs=Ab[:, T, :], start=True, stop=(T == 0))
                        for T2 in range(T):
                            r = e.matmul(ppre[k][:, 0:64], lhsT=ones_b[:], rhs=Ab[:, T2, :], start=False, stop=(T2 == T - 1))
                        return r
                    S.pe(pref_f, reads=[R], writes=[b_ppre[k]])
                    S.act(lambda e, T=T, k=k: e.copy(out=pref[:, T, :], in_=ppre[k][:, 0:64]), reads=[b_ppre[k]], writes=[prefbufs[T]])

                S.seq("dve", [
                    lambda e: e.tensor_copy(out=cnt[:], in_=pcnt[:, 0:64]),
                    lambda e: e.tensor_scalar(out=pend[:], in0=cnt[:], scalar1=127.0, scalar2=1.0 / 128, op0=ALU.add, op1=ALU.mult),
                    lambda e: e.tensor_copy(out=cntI[:], in_=pend[:]),
                    lambda e: e.tensor_copy(out=padded[:], in_=cntI[:]),
                    lambda e: e.tensor_tensor(out=pstart[:], in0=padded[:], in1=pend[:], op=ALU.is_gt),
                    lambda e: e.tensor_tensor(out=padded[:], in0=padded[:], in1=pstart[:], op=ALU.subtract),
                    lambda e: e.tensor_scalar(out=padded[:], in0=padded[:], scalar1=128.0, scalar2=None, op0=ALU.mult),
                    lambda e: e.tensor_tensor_scan(out=pend[:], data0=ones_f[:, 0:64], data1=padded[:], initial=0.0, op0=ALU.mult, op1=ALU.add),
                    lambda e: e.tensor_tensor(out=pstart[:], in0=pend[:], in1=padded[:], op=ALU.subtract),
                    lambda e: e.tensor_tensor(out=pref[:], in0=pref[:], in1=bcmid(pstart[:, :], NT), op=ALU.add),
                    lambda e: e.tensor_tensor(out=A0[:], in0=A0[:], in1=pref[:], op=ALU.mult),
                    lambda e: e.tensor_reduce(out=dest[:, 0, :], in_=A0[:], axis=AX.X, op=ALU.add),
                    lambda e: e.tensor_tensor(out=A1[:], in0=A1[:], in1=pref[:], op=ALU.mult),
                    lambda e: e.tensor_reduce(out=dest[:, 1, :], in_=A1[:], axis=AX.X, op=ALU.add),
                    lambda e: e.tensor_copy(out=destI[:], in_=dest[:]),
                ], reads=[R, b_pcnt] + prefbufs, writes=[R])

                def r5(e):
                    e.iota(bvalI[:], pattern=[[128, 128]], base=0, channel_multiplier=0)
                    return e.iota(pidxI[:], pattern=[[0, 1]], base=0, channel_multiplier=1)
                b_iota = S.buf("iota")
                S.pool(r5, writes=[b_iota])

                S.seq("dve", [
                    lambda e: e.tensor_copy(out=bval[:], in_=bvalI[:]),
                    lambda e: e.tensor_copy(out=pidx[:], in_=pidxI[:]),
                    lambda e: e.tensor_tensor(out=cmp[:], in0=bcmid(pend[:, :], 128), in1=bc3(bval[:, :], 64), op=ALU.is_le),
                    lambda e: e.tensor_reduce(out=blkE[:], in_=cmp[:], axis=AX.X, op=ALU.add),
                    lambda e: e.tensor_scalar(out=blkE[:], in0=blkE[:], scalar1=63.0, scalar2=None, op0=ALU.min),
                    lambda e: e.tensor_scalar(out=IDXf[:], in0=blkE[:], scalar1=128.0, scalar2=None, op0=ALU.mult),
                    lambda e: e.tensor_scalar(out=IDXf[:], in0=IDXf[:], scalar1=pidx[:, 0:1], scalar2=None, op0=ALU.add),
                    lambda e: e.tensor_copy(out=IDX[:], in_=IDXf[:]),
                ], reads=[R, b_iota], writes=[R])
                if debug:
                    def rdbg(e):
                        e.tensor_copy(out=tmp64[:, :, 0, 0:1], in_=dest[:, 0, :].unsqueeze(2))
                        e.tensor_copy(out=tmp64[:, :, 0, 1:2], in_=dest[:, 1, :].unsqueeze(2))
                        e.tensor_copy(out=tmp64[:, :, 0, 2:3], in_=W1[:].unsqueeze(2))
                        e.tensor_copy(out=tmp64[:, :, 0, 3:4], in_=W2[:].unsqueeze(2))
                        e.tensor_copy(out=tmp64[:, :, 0, 4:5], in_=blkE[:, 0:NT].unsqueeze(2))
                        e.tensor_copy(out=tmp64[:, :, 0, 5:6], in_=blkE[:, 32:64].unsqueeze(2))
                        e.tensor_copy(out=tmp64[:, :, 0, 6:7], in_=cnt[:, 0:NT].unsqueeze(2))
                        return e.tensor_copy(out=tmp64[:, :, 0, 7:8], in_=cnt[:, 32:64].unsqueeze(2))
                    S.dve(rdbg, reads=[R], writes=[R])
                    S.dma("sp", lambda e: e.dma_start(out=RT, in_=tmp64[:, :, 0, :]), reads=[R], writes=[S.dout("rt")])

                h2s = [sb6(f"h2s{i}", [128, 1024], F32) for i in range(2)]
                b_h2s = S.bufs(2, "h2s")
                b_xbuf = S.dout("xbuf")
                for T in range(NT):
                    u = T % 2
                    S.dma("sp", lambda e, u=u, T=T: e.dma_start(out=h2s[u][:], in_=H2[T * 128:(T + 1) * 128, :]), writes=[b_h2s[u]])
                    for k in range(2):
                        S.dma("pool", lambda e, u=u, T=T, k=k: e.indirect_dma_start(
                            out=XBUF, out_offset=bass.IndirectOffsetOnAxis(ap=destI[:, k, T:T + 1], axis=0), in_=h2s[u][:], in_offset=None),
                            reads=[b_h2s[u], R], writes=[b_xbuf], waw=False)
                S.flush()

            with contextlib.ExitStack() as st7:
                def sb7(name, shape, dt):
                    return st7.enter_context(nc.sbuf_tensor(name, list(shape), dt))

                def ps7(name, shape, dt):
                    return st7.enter_context(nc.psum_tensor(name, list(shape), dt))
                R2 = S.buf("R2")
                wg = [sb7(f"wg{i}", [128, 4096], F32) for i in range(2)]
                wu = [sb7(f"wu{i}", [128, 4096], F32) for i in range(2)]
                wdn = [sb7(f"wd{i}", [128, 4096], F32) for i in range(2)]
                b_wg = S.bufs(2, "wg")
                b_wu = S.bufs(2, "wu")
                b_wd = S.bufs(2, "wd")
                wgb = [sb7(f"wgb{i}", [128, 4096], BF16) for i in range(2)]
                wub = [sb7(f"wub{i}", [128, 4096], BF16) for i in range(2)]
                wdb = [sb7(f"wdb{i}", [128, 4096], BF16) for i in range(2)]
                b_wgb = S.bufs(2, "wgb")
                b_wub = S.bufs(2, "wub")
                b_wdbA = S.bufs(2, "wdbA")
                b_wdbB = S.bufs(2, "wdbB")
                xbt = [sb7(f"xbt{i}", [128, 1024], F32) for i in range(2)]
                b_xbt = S.bufs(2, "xbt")
                xbT = sb7("xbT", [128, 8, 128], BF16)
                b_xbT = S.buf("xbT")
                sg = sb7("sg", [128, 512], F32)
                hid = sb7("hid", [128, 512], F32)
                hdT = sb7("hdT", [128, 4, 128], BF16)
                b_sg = S.buf("sg")
                b_hid = S.buf("hid")
                b_hdT = S.buf("hdT")
                yb = [sb7(f"yb{i}", [128, 1024], F32) for i in range(2)]
                b_yb = S.bufs(2, "yb")
                pxt = ps7("pxt", [128, 8, 128], F32)
                b_pxt = S.buf("pxt")
                pg = ps7("pg", [128, 512], F32)
                pu = ps7("pu", [128, 512], F32)
                b_pg = S.buf("pg")
                b_pu = S.buf("pu")
                pht = ps7("pht", [128, 4, 128], F32)
                b_pht = S.buf("pht")
                pyd = [ps7(f"pyd{i}", [128, 512], F32) for i in range(2)]
                b_pyd = S.bufs(2, "pyd")
                b_ybuf = S.dout("ybuf")
                for b in range(128):
                    u = b % 2
                    for (wsrc, wt, bw) in ((w_g, wg, b_wg), (w_u, wu, b_wu), (w_d, wdn, b_wd)):
                        S.dma("pool", lambda e, wsrc=wsrc, wt=wt, u=u, b=b: e.indirect_dma_start(
                            out=wt[u][:], out_offset=None, in_=wsrc, in_offset=bass.IndirectOffsetOnAxis(ap=IDX[:, b:b + 1], axis=0)),
                            reads=[R2], writes=[bw[u]])
                    S.dma("sp", lambda e, u=u, b=b: e.dma_start(out=xbt[u][:], in_=XBUF[b * 128:(b + 1) * 128, :]), reads=[R2], writes=[b_xbt[u]])
                    S.act(lambda e, u=u: e.copy(out=wgb[u][:], in_=wg[u][:]), reads=[b_wg[u]], writes=[b_wgb[u]])
                    S.dve(lambda e, u=u: e.tensor_copy(out=wub[u][:], in_=wu[u][:]), reads=[b_wu[u]], writes=[b_wub[u]])
                    S.dve(lambda e, u=u: e.tensor_copy(out=wdb[u][:, 0:2048], in_=wdn[u][:, 0:2048]), reads=[b_wd[u]], writes=[b_wdbA[u]])
                    S.act(lambda e, u=u: e.copy(out=wdb[u][:, 2048:4096], in_=wdn[u][:, 2048:4096]), reads=[b_wd[u]], writes=[b_wdbB[u]])

                    def tpx(e, u=u):
                        r = None
                        for kc in range(8):
                            r = e.transpose(out=pxt[:, kc, :], in_=xbt[u][:, kc * 128:(kc + 1) * 128], identity=ident_f[:])
                        return r
                    S.pe(tpx, reads=[b_xbt[u]], writes=[b_pxt])
                    def xbcp(e):
                        e.copy(out=xbT[:, 0:4, :], in_=pxt[:, 0:4, :])
                        return e.copy(out=xbT[:, 4:8, :], in_=pxt[:, 4:8, :])
                    S.act(xbcp, reads=[b_pxt], writes=[b_xbT])

                    def gu(e, u=u):
                        r = None
                        for kc in range(8):
                            r = e.matmul(pg[:], lhsT=xbT[:, kc, :], rhs=wgb[u][:, kc * 512:(kc + 1) * 512], start=(kc == 0), stop=(kc == 7))
                        for kc in range(8):
                            r = e.matmul(pu[:], lhsT=xbT[:, kc, :], rhs=wub[u][:, kc * 512:(kc + 1) * 512], start=(kc == 0), stop=(kc == 7))
                        return r
                    S.pe(gu, reads=[b_xbT, b_wgb[u], b_wub[u]], writes=[b_pg, b_pu])
                    S.act(lambda e: e.activation(out=sg[:], in_=pg[:], func=AF.Silu), reads=[b_pg], writes=[b_sg])
                    S.dve(lambda e: e.tensor_tensor(out=hid[:], in0=pu[:], in1=sg[:], op=ALU.mult), reads=[b_pu, b_sg], writes=[b_hid])

                    def tphd(e):
                        r = None
                        for kc in range(4):
                            r = e.transpose(out=pht[:, kc, :], in_=hid[:, kc * 128:(kc + 1) * 128], identity=ident_f[:])
                        return r
                    S.pe(tphd, reads=[b_hid], writes=[b_pht])
                    S.act(lambda e: e.copy(out=hdT[:], in_=pht[:]), reads=[b_pht], writes=[b_hdT])
                    for half in range(2):
                        def dn(e, u=u, half=half):
                            r = None
                            for kc in range(4):
                                r = e.matmul(pyd[half][:], lhsT=hdT[:, kc, :], rhs=wdb[u][:, kc * 1024 + half * 512:kc * 1024 + half * 512 + 512],
                                             start=(kc == 0), stop=(kc == 3))
                            return r
                        S.pe(dn, reads=[b_hdT, b_wdbA[u], b_wdbB[u]], writes=[b_pyd[half]])
                        if half == 0:
                            S.dve(lambda e, u=u: e.tensor_copy(out=yb[u][:, 0:512], in_=pyd[0][:]), reads=[b_pyd[0]], writes=[b_yb[u]])
                        else:
                            S.act(lambda e, u=u: e.copy(out=yb[u][:, 512:1024], in_=pyd[1][:]), reads=[b_pyd[1]], writes=[b_yb[u]])
                    S.dma("sp", lambda e, u=u, b=b: e.dma_start(out=YBUF[b * 128:(b + 1) * 128, :], in_=yb[u][:]), reads=[b_yb[u]], writes=[b_ybuf], waw=False)
                S.flush()

            with contextlib.ExitStack() as st8:
                def sb8(name, shape, dt):
                    return st8.enter_context(nc.sbuf_tensor(name, list(shape), dt))
                R3 = S.buf("R3")
                fnb = sb8("fnb", [128, 1024], F32)
                b_fnb = S.buf("fnb")
                S.dma("sp", lambda e: e.dma_start(out=fnb[:], in_=fnw.partition_broadcast(128)), writes=[b_fnb])
                y0 = [sb8(f"y0_{i}", [128, 1024], F32) for i in range(2)]
                y1 = [sb8(f"y1_{i}", [128, 1024], F32) for i in range(2)]
                xx = [sb8(f"xx{i}", [128, 1024], F32) for i in range(2)]
                b_y0 = S.bufs(2, "y0")
                b_y1 = S.bufs(2, "y1")
                b_xx = S.bufs(2, "xx")
                acc = [sb8(f"acc{i}", [128, 1024], F32) for i in range(2)]
                b_acc = S.bufs(2, "acc")
                junk8 = sb8("junk8", [128, 1024], BF16)
                b_junk8 = S.buf("junk8")
                st8s = sb8("st8s", [128, NT, 4], F32)
                b_st8 = S.bufs(NT, "st8")
                ot = [sb8(f"ot{i}", [128, 1024], F32) for i in range(2)]
                b_ot = S.bufs(2, "ot")
                b_out = S.dout("out")
                for T in range(NT):
                    u = T % 2
                    S.dma("pool", lambda e, u=u, T=T: e.indirect_dma_start(out=y0[u][:], out_offset=None, in_=YBUF,
                                                                            in_offset=bass.IndirectOffsetOnAxis(ap=destI[:, 0, T:T + 1], axis=0)),
                          reads=[R3], writes=[b_y0[u]])
                    S.dma("pool", lambda e, u=u, T=T: e.indirect_dma_start(out=y1[u][:], out_offset=None, in_=YBUF,
                                                                            in_offset=bass.IndirectOffsetOnAxis(ap=destI[:, 1, T:T + 1], axis=0)),
                          reads=[R3], writes=[b_y1[u]])
                    S.dma("sp", lambda e, u=u, T=T: e.dma_start(out=xx[u][:], in_=X1[T * 128:(T + 1) * 128, :]), writes=[b_xx[u]])
                    S.dve(lambda e, u=u, T=T: e.scalar_tensor_tensor(out=acc[u][:], in0=y0[u][:], scalar=W1[:, T:T + 1], in1=xx[u][:], op0=ALU.mult, op1=ALU.add),
                          reads=[b_y0[u], b_xx[u], R3], writes=[b_acc[u]])
                    S.dve(lambda e, u=u, T=T: e.scalar_tensor_tensor(out=acc[u][:], in0=y1[u][:], scalar=W2[:, T:T + 1], in1=acc[u][:], op0=ALU.mult, op1=ALU.add),
                          reads=[b_y1[u], b_acc[u], R3], writes=[b_acc[u]])
                    S.act(lambda e, u=u, T=T: e.activation(out=junk8[:], in_=acc[u][:], func=AF.Square, accum_out=st8s[:, T, 0:1]),
                          reads=[b_acc[u]], writes=[b_junk8, b_st8[T]])
                    S.dve(lambda e, T=T: e.tensor_scalar(out=st8s[:, T, 1:2], in0=st8s[:, T, 0:1], scalar1=1.0 / D, scalar2=1e-6, op0=ALU.mult, op1=ALU.add),
                          reads=[b_st8[T]], writes=[b_st8[T]])
                    S.act(lambda e, T=T: e.activation(out=st8s[:, T, 2:3], in_=st8s[:, T, 1:2], func=AF.Sqrt), reads=[b_st8[T]], writes=[b_st8[T]])
                    S.dve(lambda e, T=T: e.reciprocal(out=st8s[:, T, 3:4], in_=st8s[:, T, 2:3]), reads=[b_st8[T]], writes=[b_st8[T]])
                    S.dve(lambda e, u=u, T=T: e.tensor_scalar(out=acc[u][:], in0=acc[u][:], scalar1=st8s[:, T, 3:4], scalar2=None, op0=ALU.mult),
                          reads=[b_acc[u], b_st8[T]], writes=[b_acc[u]])
                    S.pool(lambda e, u=u: e.tensor_tensor(out=ot[u][:], in0=acc[u][:], in1=fnb[:], op=ALU.mult), reads=[b_acc[u], b_fnb], writes=[b_ot[u]])
                    S.dma("sp", lambda e, u=u, T=T: e.dma_start(out=out[T * 128:(T + 1) * 128, :], in_=ot[u][:]), reads=[b_ot[u]], writes=[b_out], waw=False)
                S.flush(final=True)
    return nc, S


def prep_inputs(inp):
    f32 = np.float32
    w_in = np.asarray(inp["w_in"], f32)[0]
    offs = np.cumsum([0, 1024, 1024, 1024, 2048, 4096, 32, 1024, 1024])
    wq, wk, wv, wz, wx, wdt, wga, wgs = [w_in[:, offs[i]:offs[i + 1]] for i in range(8)]
    perm = np.arange(1024).reshape(8, 2, 64)
    perm = np.concatenate([perm[:, :, 32:], perm[:, :, :32]], axis=2).reshape(-1)

    def inter(w):
        ws = w[:, perm]
        return np.concatenate([np.concatenate([w[:, h * 128:(h + 1) * 128], ws[:, h * 128:(h + 1) * 128]], axis=1) for h in range(8)], axis=1)
    pad = np.zeros((1024, WCOLS - (2048 * 2 + 4096 + 1024 + 2048 + 2048 + 32)), f32)
    w2 = np.ascontiguousarray(np.concatenate([inter(wq), inter(wk), wx, wv, wz, wga, wgs, wdt, pad], axis=1))
    assert w2.shape == (1024, WCOLS), w2.shape
    com = {}
    com["w_in"] = w2
    com["normw1"] = np.ascontiguousarray(np.asarray(inp["norm_mix_w"], f32)[0].reshape(8, 128).T)
    com["convw"] = np.ascontiguousarray(np.asarray(inp["conv_w"], f32)[0].reshape(4, 32, 128).transpose(2, 1, 0))
    com["convb"] = np.ascontiguousarray(np.asarray(inp["conv_b"], f32)[0].reshape(32, 128).T)
    com["dt_bias"] = np.ascontiguousarray(np.asarray(inp["dt_bias"], f32)[0])
    com["a_log"] = np.ascontiguousarray(np.asarray(inp["a_log"], f32)[0])
    com["d_skip"] = np.ascontiguousarray(np.asarray(inp["d_skip"], f32)[0])
    com["ssd_nw"] = np.ascontiguousarray(np.asarray(inp["ssd_norm_w"], f32)[0])
    com["lam4"] = np.ascontiguousarray(np.concatenate([np.asarray(inp[k], f32)[0] for k in ("lambda_q1", "lambda_k1", "lambda_q2", "lambda_k2")]))
    com["subln"] = np.ascontiguousarray(np.asarray(inp["subln_w"], f32)[0])
    com["w_pa"] = np.ascontiguousarray(np.asarray(inp["w_branch_attn"], f32)[0])
    com["w_ps"] = np.ascontiguousarray(np.asarray(inp["w_branch_ssd"], f32)[0])
    com["w_o"] = np.ascontiguousarray(np.asarray(inp["w_out"], f32)[0])
    com["normw2"] = np.ascontiguousarray(np.asarray(inp["norm_ffn_w"], f32)[0])
    com["w_r"] = np.ascontiguousarray(np.concatenate([np.asarray(inp["w_group_router"], f32)[0], np.asarray(inp["w_expert_router"], f32)[0]], axis=1))
    com["b_r"] = np.ascontiguousarray(np.concatenate([np.asarray(inp["b_group_router"], f32)[0], np.asarray(inp["b_expert_router"], f32)[0]]))
    com["w_g"] = np.ascontiguousarray(np.asarray(inp["w_expert_gate"], f32)[0].reshape(64, 8, 128, 512).transpose(0, 2, 1, 3)).reshape(8192, 4096)
    com["w_u"] = np.ascontiguousarray(np.asarray(inp["w_expert_up"], f32)[0].reshape(64, 8, 128, 512).transpose(0, 2, 1, 3)).reshape(8192, 4096)
    com["w_d"] = np.ascontiguousarray(np.asarray(inp["w_expert_down"], f32)[0].reshape(64, 4, 128, 1024).transpose(0, 2, 1, 3)).reshape(8192, 4096)
    com["fnw"] = np.ascontiguousarray(np.asarray(inp["final_norm_w"], f32))
    p = np.arange(128)
    invf = (1.0 / (10000.0 ** (np.arange(0, 64, 2, dtype=np.float32) / 64.0))).astype(f32)
    sgn = np.where((p % 64) < 32, -1.0, 1.0).astype(f32)
    com["rconst"] = np.ascontiguousarray(np.stack([invf[p % 32], sgn], axis=1).astype(f32))
    xs = np.asarray(inp["x"], f32)
    ps_ = np.asarray(inp["positions"], np.int32)
    maps = []
    for c in range(NCORES):
        m = dict(com)
        m["x"] = np.ascontiguousarray(xs[2 * c:2 * c + 2].reshape(TOK, D))
        m["pos"] = np.ascontiguousarray(ps_[2 * c:2 * c + 2].reshape(TOK))
        maps.append(m)
    return maps


_CACHE = {}


def kernel(**inputs):
    maps = prep_inputs(inputs)
    if "nc" not in _CACHE:
        _CACHE["nc"] = build_program()[0]
    nc = _CACHE["nc"]
    res = run_bass_kernel_spmd(nc, maps, core_ids=list(range(NCORES)))
    outs = [np.asarray(r["out"], np.float32).reshape(2, 2048, D) for r in res.results]
    return np.concatenate(outs, axis=0)
```
